# Optimizing a Trainium2 kernel written in Bass

```python
import math
import jax, jax.numpy as jnp
from jax import lax
import numpy as np


D_MODEL = 1024
BATCH = 16
SEQ = 2048
DEPTH = 2

MIX_WIDTH = D_MODEL
MLSTM_WIDTH = MIX_WIDTH // 2
MLSTM_DV = 128
N_MLSTM_HEADS = MLSTM_WIDTH // MLSTM_DV
MLSTM_DK = MLSTM_DV // 2
MLSTM_CONV_W = 4
CHUNK = 64
DIFF_WIDTH = MIX_WIDTH - MLSTM_WIDTH
DIFF_HEAD_DIM = 64
DIFF_V_DIM = 2 * DIFF_HEAD_DIM
N_DIFF_HEADS = DIFF_WIDTH // DIFF_V_DIM
Q_BLOCK = 128
N_BUCKETS = 32
MAX_DISTANCE = 128
D_FF = 2816
FFN_CONV_W = 3
PLE_DIM = 256
EPS = 1e-6

COL_SIZES = [
    N_MLSTM_HEADS * MLSTM_DK,
    N_MLSTM_HEADS * MLSTM_DK,
    N_MLSTM_HEADS * MLSTM_DV,
    N_MLSTM_HEADS * MLSTM_DV,
    N_MLSTM_HEADS,
    N_MLSTM_HEADS,
    N_DIFF_HEADS * 2 * DIFF_HEAD_DIM,
    N_DIFF_HEADS * 2 * DIFF_HEAD_DIM,
    N_DIFF_HEADS * DIFF_V_DIM,
]
IN_COLS = sum(COL_SIZES)

kernel_name = 'hybrid_mlstm_diffattn_block'


def rms_norm(x, g):
    xf = x.astype(jnp.float32)
    y = xf * lax.rsqrt(jnp.mean(xf * xf, axis=-1, keepdims=True) + EPS)
    return (y * g.astype(jnp.float32)).astype(x.dtype)


def causal_dwconv(x, w):
    width, ch = w.shape
    return lax.conv_general_dilated(
        x, w.astype(x.dtype)[:, None, :], window_strides=(1,),
        padding=[(width - 1, 0)], dimension_numbers=('NWC', 'WIO', 'NWC'),
        feature_group_count=ch)


def t5_causal_bucket(dist):
    n = jnp.maximum(dist, 0)
    max_exact = N_BUCKETS // 2
    nf = jnp.maximum(n, 1).astype(jnp.float32)
    large = max_exact + (jnp.log(nf / max_exact) / math.log(MAX_DISTANCE / max_exact)
                         * (N_BUCKETS - max_exact)).astype(jnp.int32)
    large = jnp.minimum(large, N_BUCKETS - 1)
    return jnp.where(n < max_exact, n, large)


def mlstm_chunkwise(q, k, v, i_pre, f_pre):
    B, S, H, DK = q.shape
    DV = v.shape[-1]
    NC = S // CHUNK
    f32 = jnp.float32

    def to_chunks(t):
        t = t.astype(f32).reshape((B, NC, CHUNK, H) + t.shape[3:])
        return jnp.moveaxis(t, 3, 1)

    q = to_chunks(q)
    k = to_chunks(k) * (DK ** -0.5)
    v = to_chunks(v)
    ig = to_chunks(i_pre)
    logf = jax.nn.log_sigmoid(to_chunks(f_pre))
    b = jnp.cumsum(logf, axis=-1)
    g = b[..., -1]
    a = g[..., None] - b + ig

    def step(carry, inp):
        C, n, m = carry
        k_c, v_c, a_c, g_c = inp
        m_new = jnp.maximum(g_c + m, jnp.max(a_c, axis=-1))
        decay = jnp.exp(g_c + m - m_new)
        w = jnp.exp(a_c - m_new[..., None])
        C_new = decay[..., None, None] * C + jnp.einsum('bhlv,bhlk->bhvk', v_c * w[..., None], k_c)
        n_new = decay[..., None] * n + jnp.einsum('bhl,bhlk->bhk', w, k_c)
        return (C_new, n_new, m_new), (C, n, m)

    init = (jnp.zeros((B, H, DV, DK), f32), jnp.zeros((B, H, DK), f32), jnp.zeros((B, H), f32))
    xs = (jnp.moveaxis(k, 2, 0), jnp.moveaxis(v, 2, 0), jnp.moveaxis(a, 2, 0), jnp.moveaxis(g, 2, 0))
    _, (C_prev, n_prev, m_prev) = lax.scan(step, init, xs)
    C_prev = jnp.moveaxis(C_prev, 0, 2)
    n_prev = jnp.moveaxis(n_prev, 0, 2)
    m_prev = jnp.moveaxis(m_prev, 0, 2)

    e = b + m_prev[..., None]
    causal = jnp.tril(jnp.ones((CHUNK, CHUNK), dtype=bool))
    D = jnp.where(causal, b[..., :, None] - b[..., None, :] + ig[..., None, :], -jnp.inf)
    m_out = jnp.maximum(e, jnp.max(D, axis=-1))
    s = jnp.einsum('bhcld,bhcsd->bhcls', q, k) * jnp.exp(D - m_out[..., None])
    inter = jnp.exp(e - m_out)
    num = inter[..., None] * jnp.einsum('bhcld,bhcvd->bhclv', q, C_prev) \
        + jnp.einsum('bhcls,bhcsv->bhclv', s, v)
    den = inter * jnp.einsum('bhcld,bhcd->bhcl', q, n_prev) + jnp.sum(s, axis=-1)
    h = num / jnp.maximum(jnp.abs(den), jnp.exp(-m_out))[..., None]
    return jnp.moveaxis(h, 1, 3).reshape(B, S, H, DV)


def diff_attention(q, k, v, positions, rel_bias, lam, lam_init, subln_g):
    B, S, H, _, d = q.shape
    scale = d ** -0.5
    qh = jnp.transpose(q, (0, 2, 3, 1, 4))
    kh = jnp.transpose(k, (0, 2, 3, 1, 4))
    vh = jnp.transpose(v, (0, 2, 1, 3))
    outs = []
    for blk in range(S // Q_BLOCK):
        s0 = blk * Q_BLOCK
        s1 = s0 + Q_BLOCK
        qb = qh[:, :, :, s0:s1]
        kb = kh[:, :, :, :s1]
        vb = vh[:, :, :s1]
        logits = jnp.einsum('bhmqd,bhmkd->bhmqk', qb, kb).astype(jnp.float32) * scale
        bucket = t5_causal_bucket(positions[s0:s1, None] - positions[None, :s1])
        bias = jnp.transpose(rel_bias[bucket], (2, 3, 0, 1)).astype(jnp.float32)
        causal = jnp.arange(s0, s1)[:, None] >= jnp.arange(s1)[None, :]
        logits = jnp.where(causal, logits + bias, -jnp.inf)
        probs = jax.nn.softmax(logits, axis=-1)
        attn = probs[:, :, 0] - lam * probs[:, :, 1]
        outs.append(jnp.einsum('bhqk,bhkv->bhqv', attn, vb.astype(jnp.float32)))
    o = jnp.concatenate(outs, axis=2)
    o = rms_norm(o, subln_g) * (1.0 - lam_init)
    return jnp.transpose(o, (0, 2, 1, 3)).reshape(B, S, H * v.shape[-1])


def setup_inputs(seed: int = 0) -> dict:
    key = jax.random.key(seed)
    ks = jax.random.split(key, 26)
    f32 = jnp.float32

    def nrm(k, shape, scale):
        return jax.random.normal(k, shape, f32) * scale

    def gain(k, shape):
        return 1.0 + 0.05 * jax.random.normal(k, shape, f32)

    qk_cols = 2 * N_MLSTM_HEADS * MLSTM_DK
    return {
        'x': nrm(ks[0], (BATCH, SEQ, D_MODEL), 1.0),
        'p': nrm(ks[1], (DEPTH, BATCH, SEQ, PLE_DIM), 1.0),
        'positions': jnp.arange(SEQ, dtype=jnp.int32),
        'rel_bias': nrm(ks[2], (N_BUCKETS, N_DIFF_HEADS, 2), 0.2),
        'ln_mix_g': gain(ks[3], (DEPTH, D_MODEL)),
        'w_in': nrm(ks[4], (DEPTH, D_MODEL, IN_COLS), D_MODEL ** -0.5),
        'mlstm_conv_w': nrm(ks[5], (DEPTH, MLSTM_CONV_W, qk_cols), MLSTM_CONV_W ** -0.5),
        'b_igate': nrm(ks[6], (DEPTH, N_MLSTM_HEADS), 0.1),
        'b_fgate': 3.0 + nrm(ks[7], (DEPTH, N_MLSTM_HEADS), 0.5),
        'mlstm_norm_g': gain(ks[8], (DEPTH, MLSTM_DV)),
        'q_norm_g': gain(ks[9], (DEPTH, DIFF_HEAD_DIM)),
        'k_norm_g': gain(ks[10], (DEPTH, DIFF_HEAD_DIM)),
        'lam_q1': nrm(ks[11], (DEPTH, DIFF_HEAD_DIM), 0.1),
        'lam_k1': nrm(ks[12], (DEPTH, DIFF_HEAD_DIM), 0.1),
        'lam_q2': nrm(ks[13], (DEPTH, DIFF_HEAD_DIM), 0.1),
        'lam_k2': nrm(ks[14], (DEPTH, DIFF_HEAD_DIM), 0.1),
        'diff_subln_g': gain(ks[15], (DEPTH, DIFF_V_DIM)),
        'w_out': nrm(ks[16], (DEPTH, MIX_WIDTH, D_MODEL), MIX_WIDTH ** -0.5),
        'ln_ffn_g': gain(ks[17], (DEPTH, D_MODEL)),
        'w_up': nrm(ks[18], (DEPTH, D_MODEL, 2 * D_FF), D_MODEL ** -0.5),
        'ffn_conv_w': nrm(ks[19], (DEPTH, FFN_CONV_W, 2 * D_FF), FFN_CONV_W ** -0.5),
        'ffn_conv_b': nrm(ks[20], (DEPTH, 2 * D_FF), 0.02),
        'w_down': nrm(ks[21], (DEPTH, D_FF, D_MODEL), D_FF ** -0.5),
        'ln_ple_g': gain(ks[22], (DEPTH, D_MODEL)),
        'w_ple_gate': nrm(ks[23], (DEPTH, D_MODEL, D_MODEL), D_MODEL ** -0.5),
        'w_ple_proj': nrm(ks[24], (DEPTH, PLE_DIM, D_MODEL), PLE_DIM ** -0.5),
    }


def reference(x, p, positions, rel_bias, ln_mix_g, w_in, mlstm_conv_w, b_igate, b_fgate,
              mlstm_norm_g, q_norm_g, k_norm_g, lam_q1, lam_k1, lam_q2, lam_k2,
              diff_subln_g, w_out, ln_ffn_g, w_up, ffn_conv_w, ffn_conv_b, w_down,
              ln_ple_g, w_ple_gate, w_ple_proj):
    B, S, _ = x.shape
    split_idx = [int(c) for c in np.cumsum(COL_SIZES)[:-1]]
    qk_cols = N_MLSTM_HEADS * MLSTM_DK
    h = x
    for i in range(DEPTH):
        u = rms_norm(h, ln_mix_g[i])
        z = u @ w_in[i]
        qm, km, vm, om, im, fm, qd, kd, vd = jnp.split(z, split_idx, axis=-1)

        qk = jax.nn.silu(causal_dwconv(jnp.concatenate([qm, km], axis=-1), mlstm_conv_w[i]))
        qm, km = qk[..., :qk_cols], qk[..., qk_cols:]
        hm = mlstm_chunkwise(
            qm.reshape(B, S, N_MLSTM_HEADS, MLSTM_DK),
            km.reshape(B, S, N_MLSTM_HEADS, MLSTM_DK),
            vm.reshape(B, S, N_MLSTM_HEADS, MLSTM_DV),
            im + b_igate[i], fm + b_fgate[i])
        hm = rms_norm(hm, mlstm_norm_g[i]).astype(h.dtype)
        hm = hm.reshape(B, S, MLSTM_WIDTH) * jax.nn.sigmoid(om)

        lam_init = 0.8 - 0.6 * math.exp(-0.3 * i)
        lam = (jnp.exp(jnp.sum(lam_q1[i].astype(jnp.float32) * lam_k1[i].astype(jnp.float32)))
               - jnp.exp(jnp.sum(lam_q2[i].astype(jnp.float32) * lam_k2[i].astype(jnp.float32)))
               + lam_init)
        qd = rms_norm(qd.reshape(B, S, N_DIFF_HEADS, 2, DIFF_HEAD_DIM), q_norm_g[i])
        kd = rms_norm(kd.reshape(B, S, N_DIFF_HEADS, 2, DIFF_HEAD_DIM), k_norm_g[i])
        hd = diff_attention(qd, kd, vd.reshape(B, S, N_DIFF_HEADS, DIFF_V_DIM), positions,
                            rel_bias, lam, lam_init, diff_subln_g[i]).astype(h.dtype)

        h = h + jnp.concatenate([hm, hd], axis=-1) @ w_out[i]

        u = rms_norm(h, ln_ffn_g[i])
        up = causal_dwconv(u @ w_up[i], ffn_conv_w[i]) + ffn_conv_b[i]
        gate, val = up[..., :D_FF], up[..., D_FF:]
        h = h + (jax.nn.gelu(gate, approximate=False) * val) @ w_down[i]

        ple_gate = jax.nn.sigmoid(rms_norm(h, ln_ple_g[i]) @ w_ple_gate[i])
        h = h + ple_gate * (p[i] @ w_ple_proj[i])
    return h
```

```python
import math
import numpy as np
import concourse.bass as bass
import concourse.mybir as mybir
from concourse.bass_utils import run_bass_kernel_spmd

F32 = mybir.dt.float32
BF16 = mybir.dt.bfloat16
I32 = mybir.dt.int32
ALU = mybir.AluOpType
AF = mybir.ActivationFunctionType

D = 1024
S = 2048
DEPTH = 2
NCORE = 8
IN_COLS = 3080
C_QM, C_KM, C_VM, C_OM, C_IM, C_FM, C_QD, C_KD, C_VD = 0, 256, 512, 1024, 1536, 1540, 1544, 2056, 2568
DFF = 2816
EPS = 1e-6
NEG = -30000.0

K_ID, K_TRI, K_MNEG, K_OH = 0, 128, 256, 384
K_THR = 768
NCONST = 770
V_GMIX, V_GFFN, V_GPLE = 0, 16, 32
V_FCW = 48
V_FCB = V_FCW + 264
V_MCW = V_FCB + 88
V_GQ = V_MCW + 32
V_GK = V_GQ + 2
V_MNG = V_GK + 2
V_SLG = V_MNG + 256
V_BI = V_SLG + 256
V_BF = V_BI + 8
V_LAM = V_BF + 8
V_RB = V_LAM + 512
NV = V_RB + 8


SAME_ENGINE_INORDER = ('pe',)
KTOK_DMA = False


class Sched:
    def __init__(self, nc):
        self.nc = nc
        self.E = {'pe': nc.tensor, 'act': nc.scalar, 'dve': nc.vector, 'pool': nc.gpsimd, 'sp': nc.sync}
        self.sem = {e: nc.alloc_semaphore('s_' + e) for e in self.E}
        self.cnt = {e: 0 for e in self.E}
        self.waited = {e: {} for e in self.E}
        self.lastw = {}
        self.readers = {}
        self.dsem = {}
        self.dcnt = {}
        self.nins = 0

    def _wait(self, e, tok):
        kind, key, val = tok
        if kind == 'eng' and key == e and e in SAME_ENGINE_INORDER:
            return
        w = self.waited[e]
        k = (kind, key)
        if w.get(k, 0) >= val:
            return
        sem = self.sem[key] if kind == 'eng' else self.dsem[key]
        self.E[e].wait_ge(sem, val)
        w[k] = val

    def _sync(self, e, R, W):
        for r in R:
            t = self.lastw.get(r)
            if t:
                self._wait(e, t)
        for w in W:
            t = self.lastw.get(w)
            if t:
                self._wait(e, t)
            rd = self.readers.get(w)
            if rd:
                for (kind, key), val in rd.items():
                    self._wait(e, (kind, key, val))

    def _record(self, tok, R, W):
        for w in W:
            self.lastw[w] = tok
            self.readers[w] = {}
        k = (tok[0], tok[1])
        for r in R:
            d = self.readers.setdefault(r, {})
            if d.get(k, 0) < tok[2]:
                d[k] = tok[2]

    def op(self, e, fn, R=(), W=(), sig=True):
        self._sync(e, R, W)
        ins = fn(self.E[e])
        self.nins += 1
        if sig:
            self.cnt[e] += 1
            ins.then_inc(self.sem[e], 1)
            tok = ('eng', e, self.cnt[e])
        else:
            tok = ('eng', e, self.cnt[e] + 1)
        self._record(tok, R, W)
        return ins

    def dma(self, out, in_, R=(), W=(), key=None, q='sp', transpose=False):
        if key is None:
            key = W[0] if W else R[0]
        if key not in self.dsem:
            self.dsem[key] = self.nc.alloc_semaphore('d%d' % len(self.dsem))
            self.dcnt[key] = 0
        self._sync(q, R, W)
        self.dcnt[key] += 16
        if transpose:
            self.E[q].dma_start_transpose(out=out, in_=in_).then_inc(self.dsem[key], 16)
        else:
            self.E[q].dma_start(out=out, in_=in_).then_inc(self.dsem[key], 16)
        self.nins += 1
        self._record(('dma', key, self.dcnt[key]), R, W)

    def barrier(self):
        for e in self.E:
            for e2 in self.E:
                if e2 != e and self.cnt[e2] > 0:
                    self._wait(e, ('eng', e2, self.cnt[e2]))
            for k in self.dsem:
                self._wait(e, ('dma', k, self.dcnt[k]))

    def finish(self):
        for k in self.dsem:
            self._wait('sp', ('dma', k, self.dcnt[k]))
        for e2 in self.E:
            if e2 != 'sp' and self.cnt[e2] > 0:
                self._wait('sp', ('eng', e2, self.cnt[e2]))


class Ring:
    def __init__(self, items):
        self.items = list(items)
        self.i = 0

    def next(self):
        v = self.items[self.i % len(self.items)]
        self.i += 1
        return v


def build(nseq=2, nlayer=DEPTH, dbg=None):
    nc = bass.Bass("TRN2", target_bir_lowering=False)
    x_d = nc.dram_tensor("x", [2, S, D], F32, kind="ExternalInput")
    p_d = nc.dram_tensor("p", [DEPTH, 2, S, 256], F32, kind="ExternalInput")
    w_in_d = nc.dram_tensor("w_in", [DEPTH, D, IN_COLS], F32, kind="ExternalInput")
    w_out_d = nc.dram_tensor("w_out", [DEPTH, D, D], F32, kind="ExternalInput")
    w_up_d = nc.dram_tensor("w_up", [DEPTH, D, 2 * DFF], F32, kind="ExternalInput")
    w_dn_d = nc.dram_tensor("w_down", [DEPTH, DFF, D], F32, kind="ExternalInput")
    w_pg_d = nc.dram_tensor("w_ple_gate", [DEPTH, D, D], F32, kind="ExternalInput")
    w_pp_d = nc.dram_tensor("w_ple_proj", [DEPTH, 256, D], F32, kind="ExternalInput")
    consts_d = nc.dram_tensor("consts", [128, NCONST], F32, kind="ExternalInput")
    pos_d = nc.dram_tensor("positions", [S], I32, kind="ExternalInput")
    vecs_d = nc.dram_tensor("vecs", [128, NV], F32, kind="ExternalInput")
    out_d = nc.dram_tensor("out", [2, S, D], F32, kind="ExternalOutput")
    scr_d = nc.dram_tensor("scr", [8, 128, 384], F32, kind="Internal")
    dbg_d = None
    if dbg is not None:
        dbg_d = nc.dram_tensor("dbg", [128, 8 * S], F32, kind="ExternalOutput")

    sc = Sched(nc)
    op, dma = sc.op, sc.dma

    hT = nc.alloc_sbuf_tensor("hT", [128, 8 * S], F32)
    hT3 = hT[:, :].rearrange("p (c t) -> p c t", t=S)
    consts = nc.alloc_sbuf_tensor("consts_sb", [128, NCONST], F32)
    vecs = nc.alloc_sbuf_tensor("vecs_sb", [128, NV], F32)
    biasT = nc.alloc_sbuf_tensor("biasT", [128, 16 * 128], F32)
    wst = [nc.alloc_sbuf_tensor("wst%d" % i, [128, 2048], F32) for i in range(2)]
    wbf = [nc.alloc_sbuf_tensor("wbf%d" % i, [128, 2048], BF16) for i in range(4)]
    cb = nc.alloc_sbuf_tensor("cb", [128, 3 * 128], BF16)
    ct = nc.alloc_sbuf_tensor("ct", [128, 16], F32)
    sq = nc.alloc_sbuf_tensor("sq", [128, 8 * 512], BF16)
    sd = nc.alloc_sbuf_tensor("sd", [128, 512], F32)
    rstd = nc.alloc_sbuf_tensor("rstd", [128, 512], F32)
    sm = nc.alloc_sbuf_tensor("sm", [128, 64], F32)
    ARENA = 20800
    arena = nc.alloc_sbuf_tensor("arena", [128, ARENA], F32)
    PS = nc.alloc_psum_tensor("ps", [128, 4096], F32)

    identf = consts[:, K_ID:K_ID + 128]
    tri = consts[:, K_TRI:K_TRI + 128]
    identb = cb[:, 0:128]
    onesb = cb[:, 128:256]
    blk1 = cb[:, 256:384]
    eps_c = ct[:, 0:1]
    one_c = ct[:, 1:2]
    nhalf_c = ct[:, 2:3]
    nhalf2 = ct[:, 8:10]

    class Arena:
        def __init__(self):
            self.off = 0

        def reset(self):
            self.off = 0

        def f32(self, n):
            a = arena[:, self.off:self.off + n]
            self.off += n
            assert self.off <= ARENA, self.off
            return a

        def bf16(self, n):
            assert n % 2 == 0
            return self.f32(n // 2).bitcast(BF16)

    AR = Arena()

    def psb(bank, a=0, b=512):
        return PS[:, bank * 512 + a: bank * 512 + b]

    def vcol(c, n=1):
        return vecs[:, c:c + n]

    wring = {'st': 0, 'bf': 0}

    def loadw(parts, n, dst_ap=None, dst_res=None, eng='pool'):
        si = wring['st'] % 2
        wring['st'] += 1
        for (off, nk, ncol, src) in parts:
            dst = wst[si][:, off:off + nk * ncol].rearrange("p (k c) -> p k c", c=ncol)
            dma(dst, src.rearrange("(k p) c -> p k c", p=128), W=[('wst', si)])
        if dst_ap is not None:
            op('pool', lambda e: e.tensor_copy(out=dst_ap[:, 0:n], in_=wst[si][:, 0:n]), R=[('wst', si)], W=[dst_res])
            return None
        bi = wring['bf'] % 4
        wring['bf'] += 1
        if eng == 'act':
            op('act', lambda e: e.copy(out=wbf[bi][:, 0:n], in_=wst[si][:, 0:n]), R=[('wst', si)], W=[('wbf', bi)])
        else:
            op(eng, lambda e: e.tensor_copy(out=wbf[bi][:, 0:n], in_=wst[si][:, 0:n]), R=[('wst', si)], W=[('wbf', bi)])
        return bi

    def wv(bi, off, nk, ncol):
        return wbf[bi][:, off:off + nk * ncol].rearrange("p (k c) -> p k c", c=ncol)

    class WStream:
        def __init__(self, specs, look=2, eng='pool'):
            self.specs = specs
            self.look = look
            self.eng = eng
            self.issued = []

        def get(self, i):
            while len(self.issued) < min(len(self.specs), i + 1 + self.look):
                parts, n = self.specs[len(self.issued)]
                self.issued.append(loadw(parts, n, eng=self.eng))
            return self.issued[i]

    dma(consts[:, :], consts_d[:, :], W=['consts'])
    dma(vecs[:, :], vecs_d[:, :], W=['vecs'])
    op('pool', lambda e: e.memset(ct[:, 0:1], EPS), W=['ct'])
    op('pool', lambda e: e.memset(ct[:, 1:2], 1.0), W=['ct'])
    op('pool', lambda e: e.memset(ct[:, 2:3], -0.5), W=['ct'])
    op('pool', lambda e: e.memset(ct[:, 3:4], 0.0), W=['ct'])
    op('pool', lambda e: e.memset(ct[:, 8:10], -0.5), W=['ct'])
    op('pool', lambda e: e.tensor_copy(out=identb, in_=identf), R=['consts'], W=['cb'])
    op('pool', lambda e: e.memset(onesb, 1.0), W=['cb'])
    op('pool', lambda e: e.memset(blk1, 0.0), W=['cb'])
    op('pool', lambda e: e.memset(cb[0:64, 256:320], 1.0), W=['cb'])
    op('pool', lambda e: e.memset(cb[64:128, 320:384], 1.0), W=['cb'])
    lam_init = [0.8 - 0.6 * math.exp(-0.3 * l) for l in range(DEPTH)]
    for l in range(DEPTH):
        base = V_LAM + l * 256
        op('dve', lambda e: e.scalar_tensor_tensor(out=sd[:, 0:64], in0=vcol(base, 64), scalar=1.0, in1=vcol(base + 64, 64),
                                                   op0=ALU.mult, op1=ALU.mult, accum_out=sm[:, 0:1]), R=['vecs'], W=['sd', 'sm'])
        op('dve', lambda e: e.scalar_tensor_tensor(out=sd[:, 0:64], in0=vcol(base + 128, 64), scalar=1.0, in1=vcol(base + 192, 64),
                                                   op0=ALU.mult, op1=ALU.mult, accum_out=sm[:, 1:2]), R=['vecs'], W=['sd', 'sm'])
        op('act', lambda e: e.activation(out=sm[:, 2:4], in_=sm[:, 0:2], func=AF.Exp), R=['sm'], W=['sm2'])
        op('dve', lambda e: e.tensor_tensor(out=sm[:, 4:5], in0=sm[:, 3:4], in1=sm[:, 2:3], op=ALU.subtract), R=['sm2'], W=['sm3'])
        op('dve', lambda e: e.tensor_scalar(out=ct[:, 4 + l:5 + l], in0=sm[:, 4:5], scalar1=-lam_init[l], scalar2=None, op0=ALU.add),
           R=['sm3'], W=['ct'])
        op('pool', lambda e: e.memset(ct[:, 6 + l:7 + l], EPS / (1.0 - lam_init[l]) ** 2), W=['ct'])
    posi = arena[0:32, 0:257].bitcast(I32)
    sqf = sq[:, :].bitcast(F32)
    dma(posi, bass.AP(pos_d, 0, [[0, 32], [1, 257]]), W=['posi'])
    op('pool', lambda e: e.memset(sd[0:32, 0:127], 0.0), W=['sd'])
    op('dve', lambda e: e.tensor_copy(out=sd[0:32, 127:384], in_=posi), R=['posi'], W=['sd'])
    op('dve', lambda e: e.tensor_copy(out=sm[0:32, 8:9], in_=sd[0:32, 127:128]), R=['sd'], W=['sm'])
    op('dve', lambda e: e.tensor_scalar(out=sd[0:32, 127:384], in0=sd[0:32, 127:384], scalar1=sm[0:32, 8:9], scalar2=None, op0=ALU.subtract),
       R=['sm'], W=['sd'])
    op('dve', lambda e: e.tensor_scalar(out=sqf[0:32, 0:384], in0=sd[0:32, 0:384], scalar1=consts[0:32, K_THR:K_THR + 1], scalar2=None, op0=ALU.is_ge),
       R=['sd', 'consts'], W=['sq'])
    op('dve', lambda e: e.tensor_scalar(out=sqf[0:32, 384:768], in0=sd[0:32, 0:384], scalar1=consts[0:32, K_THR + 1:K_THR + 2], scalar2=None, op0=ALU.is_lt),
       R=['sd', 'consts'], W=['sq'])
    op('dve', lambda e: e.tensor_tensor(out=sqf[0:32, 768:1152], in0=sqf[0:32, 0:384], in1=sqf[0:32, 384:768], op=ALU.mult), R=['sq'], W=['sq'])
    op('pe', lambda e: e.matmul(PS[0:8, 0:384], lhsT=vecs[0:32, V_RB:V_RB + 8], rhs=sqf[0:32, 768:1152], start=True, stop=True),
       R=['vecs', 'sq'], W=[('ps', 0)])
    op('act', lambda e: e.copy(out=sd[0:8, 0:1], in_=PS[0:8, 383:384]), R=[('ps', 0)], W=['sd'])
    op('dve', lambda e: e.tensor_scalar(out=rstd[0:8, 0:384], in0=PS[0:8, 0:384], scalar1=sd[0:8, 0:1], scalar2=None, op0=ALU.subtract),
       R=[('ps', 0), 'sd'], W=['rstd'])
    dma(scr_d[:, :, :], bass.AP(rstd, 0, [[512, 8], [0, 128], [1, 384]]), R=['rstd'], W=['scr'], key='scr')
    for dl in range(2):
        for hc in range(8):
            src = bass.AP(scr_d, hc * 128 * 384 + 127 + 128 * dl, [[383, 128], [1, 128]])
            dma(biasT[:, (dl * 8 + hc) * 128:(dl * 8 + hc + 1) * 128], src, R=['scr'], W=['biasT'], key='biasT')
    b0v = biasT[:, 0:1024].rearrange("p (h q) -> p h q", q=128)
    op('dve', lambda e: e.tensor_tensor(out=b0v, in0=b0v, in1=bass.AP(consts, K_MNEG, [[NCONST, 128], [0, 8], [1, 128]]), op=ALU.add),
       R=['biasT', 'consts'], W=['biasT'])

    op('act', lambda e: e.activation(out=biasT[:, :], in_=biasT[:, :], func=AF.Exp), R=['biasT'], W=['biasT'])

    psX = Ring([0, 1, 2, 3])

    def phase_load(s):
        AR.reset()
        xs = [AR.f32(1024), AR.f32(1024)]
        for t in range(16):
            b = t % 2
            dma(xs[b], x_d[s, t * 128:(t + 1) * 128, :], W=[('xs', b)])
            for half in range(2):
                bank = psX.next()
                for c4 in range(4):
                    c = half * 4 + c4
                    op('pe', lambda e: e.transpose(out=psb(bank, c4 * 128, c4 * 128 + 128), in_=xs[b][:, c * 128:(c + 1) * 128], identity=identf),
                       R=[('xs', b), 'consts'], W=[('ps', bank)], sig=(c4 == 3))
                eng = 'act' if half == 0 else 'dve'
                src = psb(bank).rearrange("p (c t) -> p c t", t=128)
                dst = hT3[:, half * 4:half * 4 + 4, t * 128:(t + 1) * 128]
                if eng == 'act':
                    op('act', lambda e: e.copy(out=dst, in_=src), R=[('ps', bank)], W=[('h', t // 4)])
                else:
                    op('dve', lambda e: e.tensor_copy(out=dst, in_=src), R=[('ps', bank)], W=[('h', t // 4)])

    def phase_store(s):
        AR.reset()
        os_ = [AR.f32(1024), AR.f32(1024)]
        for t in range(16):
            b = t % 2
            for half in range(2):
                bank = psX.next()
                for c4 in range(4):
                    c = half * 4 + c4
                    op('pe', lambda e: e.transpose(out=psb(bank, c4 * 128, c4 * 128 + 128), in_=hT3[:, c, t * 128:(t + 1) * 128], identity=identf),
                       R=[('h', t // 4), 'consts'], W=[('ps', bank)], sig=(c4 == 3))
                dst = os_[b][:, half * 512:(half + 1) * 512]
                if half == 0:
                    op('act', lambda e: e.copy(out=dst, in_=psb(bank)), R=[('ps', bank)], W=[('os', b)])
                else:
                    op('dve', lambda e: e.tensor_copy(out=dst, in_=psb(bank)), R=[('ps', bank)], W=[('os', b)])
            dma(out_d[s, t * 128:(t + 1) * 128, :], os_[b], R=[('os', b)], key=('os', b))

    psN = Ring([6, 7])

    def norm(gcol, tok0, ntok, u3, ures):
        sq3 = sq[:, :].rearrange("p (c t) -> p c t", t=512)
        for b in range(ntok // 512):
            t0 = tok0 + b * 512
            hres = ('h', t0 // 512)
            op('act', lambda e: e.activation(out=sq3, in_=hT3[:, :, t0:t0 + 512], func=AF.Square), R=[hres], W=['sq'])
            bank = psN.next()
            for c in range(8):
                op('pe', lambda e: e.matmul(psb(bank), lhsT=onesb, rhs=sq3[:, c, :], start=(c == 0), stop=(c == 7)),
                   R=['sq', 'cb'], W=[('ps', bank)], sig=(c == 7))
            op('act', lambda e: e.activation(out=sd[:, :], in_=psb(bank), func=AF.Ln, bias=eps_c, scale=1.0 / D),
               R=[('ps', bank), 'ct'], W=['sd'])
            op('act', lambda e: e.activation(out=rstd[:, :], in_=sd[:, :], func=AF.Exp, scale=-0.5), R=['sd'], W=['rstd'])
            for c in range(8):
                op('dve', lambda e: e.scalar_tensor_tensor(out=u3[:, c, b * 512:(b + 1) * 512], in0=hT3[:, c, t0:t0 + 512],
                                                           scalar=vcol(gcol + c), in1=rstd[:, :], op0=ALU.mult, op1=ALU.mult),
                   R=[hres, 'rstd', 'vecs'], W=[(ures, b)])

    def tiny_rstd(dst, src, scale, eps_f, Rr, Ww, nh=None):
        op('pool', lambda e: e.tensor_scalar(out=dst, in0=src, scalar1=scale, scalar2=eps_f, op0=ALU.mult, op1=ALU.add), R=Rr, W=Ww)
        op('pool', lambda e: e.tensor_tensor(out=dst, in0=dst, in1=(nhalf_c if nh is None else nh), op=ALU.pow), R=Ww, W=Ww)

    def wout_partial(l, r0, mix3):
        specs = []
        for half in range(2):
            src = w_out_d[l, r0:r0 + 512, half * 512:(half + 1) * 512]
            specs.append(([(0, 4, 512, src)], 2048))
        ws = WStream(specs)
        ring = Ring([4, 5, 6, 7])
        for half in range(2):
            bi = ws.get(half)
            w3 = wv(bi, 0, 4, 512)
            for o4 in range(4):
                oc = half * 4 + o4
                for blk in range(4):
                    bank = ring.next()
                    for kc in range(4):
                        op('pe', lambda e: e.matmul(psb(bank), lhsT=w3[:, kc, o4 * 128:(o4 + 1) * 128], rhs=mix3[:, kc, blk * 512:(blk + 1) * 512],
                                                    start=(kc == 0), stop=(kc == 3)),
                           R=[('wbf', bi)] + [('mix', kc, 4 * blk + t_) for t_ in range(4)], W=[('ps', bank)], sig=(kc == 3))
                    dst = hT3[:, oc, blk * 512:(blk + 1) * 512]
                    op('dve', lambda e: e.tensor_tensor(out=dst, in0=psb(bank), in1=dst, op=ALU.add), R=[('ps', bank)], W=[('h', blk)])

    def mixer(l, s):
        AR.reset()
        uT = AR.bf16(8 * S)
        u3 = uT.rearrange("p (c t) -> p c t", t=S)
        mixT = AR.bf16(4 * S)
        mix3 = mixT.rearrange("p (c t) -> p c t", t=S)
        stage_off = AR.off
        specs = []
        for h in range(4):
            specs.append(([(0, 8, 128, w_in_d[l, :, C_QD + h * 128:C_QD + (h + 1) * 128]),
                           (1024, 8, 128, w_in_d[l, :, C_KD + h * 128:C_KD + (h + 1) * 128])], 2048))
            specs.append(([(0, 8, 128, w_in_d[l, :, C_VD + h * 128:C_VD + (h + 1) * 128])], 1024))
        ws = WStream(specs, look=2)
        ws.get(0)
        ws.get(1)
        norm(V_GMIX + l * 8, 0, S, u3, 'u')
        ures = [('u', b) for b in range(4)]
        if dbg == 'm1':
            return 'stop'

        QTs = [AR.bf16(S) for _ in range(2)]
        KTs = [AR.bf16(S) for _ in range(2)]
        VPs = [AR.bf16(16 * 130) for _ in range(2)]
        VP3s = [v_.rearrange("p (t c) -> p t c", c=130) for v_ in VPs]
        PT = [AR.bf16(1024) for _ in range(2)]
        TMP = [sd[:, :], rstd[:, :], PT[0].bitcast(F32), PT[1].bitcast(F32)]
        sdres = [['sd'], ['rstd'], [('pt', 0, 0), ('pt', 0, 1)], [('pt', 1, 0), ('pt', 1, 1)]]
        JK = sq[:, 2048:2176]
        YT = [AR.bf16(128) for _ in range(2)]
        ACCS = [AR.f32(258) for _ in range(4)]
        o_ring = Ring([0, 1, 2, 3])
        for hb in range(2):
            op('pool', lambda e: e.memset(VP3s[hb][:, :, 128:130], 1.0), W=[('vp1', hb)])
        scr_ring = Ring([(4, 6), (5, 7)])
        prj_ring = Ring([(0, 1), (2, 3), (4, 6), (5, 7)])
        pt_ring = Ring([0, 1])
        qn_ring = Ring([0, 1, 2, 3])
        yt_ring = Ring([0, 1])
        sm_ring = Ring([0, 1, 2, 3])
        sc_att = 1.0 / 8.0
        sub_scale = 1.0 / (128.0 * (1.0 - lam_init[l]) ** 2)

        def proj_gen(h):
            hb = h % 2
            bqk = ws.get(2 * h)
            bv = ws.get(2 * h + 1)
            wq3 = wv(bqk, 0, 8, 128)
            wk3 = wv(bqk, 1024, 8, 128)
            wv3 = wv(bv, 0, 8, 128)
            for (w3, dstT, gcol, dres) in ((wq3, QTs[hb], V_GQ + l, 'qt'), (wk3, KTs[hb], V_GK + l, 'kt')):
                for blk in range(4):
                    bank, bank2 = prj_ring.next()
                    for kc in range(8):
                        op('pe', lambda e: e.matmul(psb(bank), lhsT=w3[:, kc, :], rhs=u3[:, kc, blk * 512:(blk + 1) * 512],
                                                    start=(kc == 0), stop=(kc == 7)),
                           R=[('wbf', bqk), ures[blk]], W=[('ps', bank)], sig=(kc == 7))
                    qi = qn_ring.next()
                    sqs = sq[:, qi * 512:(qi + 1) * 512]
                    sdq = TMP[qi]
                    op('act', lambda e: e.activation(out=sqs, in_=psb(bank), func=AF.Square), R=[('ps', bank)], W=[('sqs', qi)])
                    op('pe', lambda e: e.matmul(psb(bank2), lhsT=blk1, rhs=sqs, start=True, stop=True),
                       R=[('sqs', qi), 'cb'], W=[('ps', bank2)])
                    op('act', lambda e: e.activation(out=sdq, in_=psb(bank2), func=AF.Ln, bias=eps_c, scale=1.0 / 64.0),
                       R=[('ps', bank2), 'ct'], W=sdres[qi])
                    op('act', lambda e: e.activation(out=sdq, in_=sdq, func=AF.Exp, scale=-0.5), R=sdres[qi], W=sdres[qi])
                    op('dve', lambda e: e.scalar_tensor_tensor(out=dstT[:, blk * 512:(blk + 1) * 512], in0=psb(bank), scalar=vcol(gcol),
                                                               in1=sdq, op0=ALU.mult, op1=ALU.mult),
                       R=[('ps', bank), 'vecs'] + sdres[qi], W=[(dres, hb, blk)])
                    yield
            for t4 in range(4):
                bank, _ = prj_ring.next()
                for tt in range(4):
                    t = t4 * 4 + tt
                    for kc in range(8):
                        op('pe', lambda e: e.matmul(psb(bank, tt * 128, tt * 128 + 128), lhsT=u3[:, kc, t * 128:(t + 1) * 128], rhs=wv3[:, kc, :],
                                                    start=(kc == 0), stop=(kc == 7)),
                           R=[('wbf', bv), ures[t4]], W=[('ps', bank)], sig=(kc == 7 and tt == 3))
                op('act', lambda e: e.copy(out=VP3s[hb][:, t4 * 4:t4 * 4 + 4, 0:128], in_=psb(bank).rearrange("p (t c) -> p t c", c=128)),
                   R=[('ps', bank)], W=[('vp', hb, t4)])
                yield

        for h in range(4):
            hb = h % 2
            if hb == 0:
                for _ in proj_gen(h):
                    pass
                for _ in proj_gen(h + 1):
                    pass
            QT, KT, VP3 = QTs[hb], KTs[hb], VP3s[hb]
            gnext = None
            def emit_scores(qb, jp):
                sbs = scr_ring.next()
                for comp in range(2):
                    for jj in range(2):
                        j = 2 * jp + jj
                        op('pe', lambda e: e.matmul(psb(sbs[comp], jj * 256, jj * 256 + 256),
                                                    lhsT=KT[comp * 64:(comp + 1) * 64, j * 128:(j + 1) * 128],
                                                    rhs=QT[comp * 64:(comp + 1) * 64, qb * 256:qb * 256 + 256], start=True, stop=True),
                           R=[('kt', hb, j // 4), ('qt', hb, qb // 2)], W=[('ps', sbs[comp])], sig=(jj == 1))
                return sbs

            def emit_exp(qb, jp, sbs):
                pi = pt_ring.next()
                pt = PT[pi]
                active = []
                for comp in range(2):
                    op('act', lambda e: e.activation(out=pt[:, comp * 512:comp * 512 + 512], in_=psb(sbs[comp]), func=AF.Exp, scale=sc_att),
                       R=[('ps', sbs[comp])], W=[('pt', pi, comp)])
                for jj in range(2):
                    j = 2 * jp + jj
                    for t in range(2):
                        dl = 2 * qb + t - j
                        if dl < 0:
                            continue
                        for comp in range(2):
                            if dl < 2:
                                a_ = comp * 512 + jj * 256 + t * 128
                                bt = biasT[:, (dl * 8 + h * 2 + comp) * 128:(dl * 8 + h * 2 + comp + 1) * 128]
                                op('dve', lambda e: e.tensor_tensor(out=pt[:, a_:a_ + 128], in0=pt[:, a_:a_ + 128], in1=bt, op=ALU.mult),
                                   R=['biasT'], W=[('pt', pi, comp)])
                            active.append((jj, comp, t))
                return (pi, active)

            def emit_pv(qb, jp, pi, active):
                pt = PT[pi]
                for (jj, comp, t) in active:
                    j = 2 * jp + jj
                    i = 2 * qb + t
                    ab = t * 2 + comp
                    a_ = comp * 512 + jj * 256 + t * 128
                    op('pe', lambda e: e.matmul(psb(ab, 0, 129), lhsT=pt[:, a_:a_ + 128],
                                                rhs=VP3[:, j, 0:129], start=(j == 0), stop=(j == i)),
                       R=[('pt', pi, comp), ('vp', hb, j // 4), ('vp1', hb)], W=[('ps', ab)], sig=True)

            def emit_finalize1(qb):
                out = []
                for t in range(2):
                    i = 2 * qb + t
                    a1, a2 = t * 2, t * 2 + 1
                    k0 = sm_ring.next() * 8
                    r = sm[:, k0:k0 + 8]
                    rres = ('smr', k0)
                    oi = o_ring.next()
                    ac = ACCS[oi]
                    op('dve', lambda e: e.tensor_copy(out=ac[:, 0:129], in_=psb(a1, 0, 129)), R=[('ps', a1)], W=[('accs', oi, 0)])
                    op('dve', lambda e: e.tensor_copy(out=ac[:, 129:258], in_=psb(a2, 0, 129)), R=[('ps', a2)], W=[('accs', oi, 1)])
                    op('dve', lambda e: e.reciprocal(out=r[:, 0:1], in_=ac[:, 128:129]), R=[('accs', oi, 0)], W=[rres])
                    op('dve', lambda e: e.reciprocal(out=r[:, 1:2], in_=ac[:, 257:258]), R=[('accs', oi, 1)], W=[rres])
                    op('dve', lambda e: e.tensor_tensor(out=r[:, 1:2], in0=r[:, 1:2], in1=ct[:, 4 + l:5 + l], op=ALU.mult), R=['ct'], W=[rres])
                    OOi = ac[:, 129:257]
                    op('dve', lambda e: e.tensor_scalar(out=ac[:, 0:128], in0=ac[:, 0:128], scalar1=r[:, 0:1], scalar2=None, op0=ALU.mult),
                       R=[rres], W=[('accs', oi, 0)])
                    op('dve', lambda e: e.scalar_tensor_tensor(out=OOi, in0=OOi, scalar=r[:, 1:2], in1=ac[:, 0:128], op0=ALU.mult, op1=ALU.add),
                       R=[('accs', oi, 0), rres], W=[('accs', oi, 1)])
                    op('dve', lambda e: e.scalar_tensor_tensor(out=ac[:, 0:128], in0=OOi, scalar=1.0, in1=OOi, op0=ALU.mult, op1=ALU.mult,
                                                               accum_out=r[:, 2:3]), R=[('accs', oi, 1)], W=[('accs', oi, 0), (rres, 2)])
                    tiny_rstd(r[:, 3:4], r[:, 2:3], sub_scale, EPS / (1.0 - lam_init[l]) ** 2, [(rres, 2), 'ct'], [(rres, 3)])
                    out.append((i, oi, r, rres))
                return out

            def emit_finalize2(items):
                for (i, oi, r, rres) in items:
                    OOi = ACCS[oi][:, 129:257]
                    yi = yt_ring.next()
                    op('dve', lambda e: e.scalar_tensor_tensor(out=YT[yi], in0=OOi, scalar=r[:, 3:4], in1=vcol(V_SLG + l * 128, 128),
                                                               op0=ALU.mult, op1=ALU.mult), R=[('accs', oi, 1), (rres, 3), 'vecs'], W=[('yt', yi)])
                    dma(mix3[:, h, i * 128:(i + 1) * 128], YT[yi], R=[('yt', yi)], W=[('mix', h, i)], key=('ytd', yi), transpose=True)

            steps = [(qb, jp) for qb in range(8) for jp in range(qb + 1)]
            prev = None
            pend_final = []
            pend_f2 = []
            for (qb, j) in steps:
                sbs = emit_scores(qb, j)
                if prev is not None:
                    emit_pv(*prev)
                    if prev[1] == prev[0]:
                        pend_final.append(prev[0])
                pi, active = emit_exp(qb, j, sbs)
                while pend_f2:
                    emit_finalize2(pend_f2.pop(0))
                while pend_final:
                    pend_f2.append(emit_finalize1(pend_final.pop(0)))
                prev = (qb, j, pi, active)
            emit_pv(*prev)
            while pend_f2:
                emit_finalize2(pend_f2.pop(0))
            emit_finalize2(emit_finalize1(prev[0]))
            if dbg == 'm4':
                return 'stop'
        if dbg == 'mixA':
            return mix3
        wout_partial(l, 512, mix3)

        sc.barrier()
        AR.off = stage_off
        QT = AR.bf16(S)
        KT = AR.bf16(S)
        JK = AR.f32(128)
        RAW = AR.f32(S + 32)
        ACC = sq[:, :].bitcast(F32)
        KTOK = AR.bf16(16 * 128)
        KTOK3 = KTOK.rearrange("p (t c) -> p t c", c=128)
        GI = AR.f32(64)
        SP_ = AR.f32(64)
        OM = AR.f32(64)
        PSI = AR.f32(64)
        DEC = AR.f32(64)
        NPSI = AR.f32(64)
        NPSI3 = NPSI.rearrange("p (t h) -> p t h", h=4)
        DSEL = AR.f32(16)
        STM = [AR.bf16(128) for _ in range(4)]
        RF = AR.f32(132)
        RBA = AR.bf16(16 * 132)
        RBA3 = RBA.rearrange("p (c k) -> p c k", k=132)
        HRs = [AR.f32(128) for _ in range(2)]
        YTm = [AR.bf16(128) for _ in range(2)]
        GI3 = GI.rearrange("p (t h) -> p t h", h=4)
        SP3 = SP_.rearrange("p (t h) -> p t h", h=4)
        OM3 = OM.rearrange("p (t h) -> p t h", h=4)
        PSI3 = PSI.rearrange("p (t h) -> p t h", h=4)
        DEC3 = DEC.rearrange("p (t h) -> p t h", h=4)
        bg = loadw([(0, 8, 8, w_in_d[l, :, C_IM:C_IM + 8])], 64)
        wg3 = wv(bg, 0, 8, 8)
        gb = 4
        for t in range(16):
            for kc in range(8):
                op('pe', lambda e: e.matmul(psb(gb, t * 8, t * 8 + 8), lhsT=u3[:, kc, t * 128:(t + 1) * 128], rhs=wg3[:, kc, :],
                                            start=(kc == 0), stop=(kc == 7)),
                   R=[('wbf', bg), ures[t // 4]], W=[('ps', gb)], sig=(kc == 7 and t == 15))
        pg3 = psb(gb, 0, 128).rearrange("p (t j) -> p t j", j=8)
        op('dve', lambda e: e.tensor_tensor(out=GI3, in0=pg3[:, :, 0:4], in1=bass.AP(vecs, V_BI + l * 4, [[NV, 128], [0, 16], [1, 4]]), op=ALU.add),
           R=[('ps', gb), 'vecs'], W=['gi'])
        op('dve', lambda e: e.tensor_tensor(out=SP3, in0=pg3[:, :, 4:8], in1=bass.AP(vecs, V_BF + l * 4, [[NV, 128], [0, 16], [1, 4]]), op=ALU.add),
           R=[('ps', gb), 'vecs'], W=['sp'])
        op('act', lambda e: e.activation(out=SP_, in_=SP_, func=AF.Exp, scale=-1.0), R=['sp'], W=['sp'])
        op('act', lambda e: e.activation(out=SP_, in_=SP_, func=AF.Ln, bias=one_c), R=['sp', 'ct'], W=['sp'])
        op('pool', lambda e: e.memset(sd[:, 0:128], 1.0), W=['sd'])
        op('pe', lambda e: e.matmul(psb(5, 0, 64), lhsT=tri, rhs=SP_, start=True, stop=True), R=['sp', 'consts'], W=[('ps', 5)])
        op('pe', lambda e: e.matmul(psb(6, 0, 64), lhsT=sd[:, 0:128], rhs=SP_, start=True, stop=True), R=['sp', 'sd'], W=[('ps', 6)])
        op('dve', lambda e: e.tensor_tensor(out=OM, in0=psb(5, 0, 64), in1=GI, op=ALU.add), R=[('ps', 5), 'gi'], W=['om'])
        op('act', lambda e: e.activation(out=OM, in_=OM, func=AF.Exp), R=['om'], W=['om'])
        op('act', lambda e: e.activation(out=PSI, in_=psb(5, 0, 64), func=AF.Exp, scale=-1.0), R=[('ps', 5)], W=['psi'])
        op('act', lambda e: e.activation(out=DEC, in_=psb(6, 0, 64), func=AF.Exp, scale=-1.0), R=[('ps', 6)], W=['dec'])
        op('dve', lambda e: e.tensor_scalar(out=NPSI, in0=PSI, scalar1=-1.0, scalar2=None, op0=ALU.mult), R=['psi'], W=['psi'])

        for hp in range(2):
            op('pool', lambda e: e.memset(RAW[:, 0:3], 0.0), W=['raw', 'sq'] + [('vpa', c_) for c_ in range(16)] + [('oga', c_) for c_ in range(16)])
            bqk = loadw([(0, 8, 128, w_in_d[l, :, C_QM + hp * 128:C_QM + (hp + 1) * 128]),
                         (1024, 8, 128, w_in_d[l, :, C_KM + hp * 128:C_KM + (hp + 1) * 128])], 2048)
            bvv = loadw([(0, 8, 256, w_in_d[l, :, C_VM + hp * 256:C_VM + (hp + 1) * 256])], 2048)
            boo = loadw([(0, 8, 256, w_in_d[l, :, C_OM + hp * 256:C_OM + (hp + 1) * 256])], 2048)
            wq3 = wv(bqk, 0, 8, 128)
            wk3 = wv(bqk, 1024, 8, 128)
            wv3 = wv(bvv, 0, 8, 256)
            wo3 = wv(boo, 0, 8, 256)
            pr = Ring([4, 5])
            for (w3, dstT, chunk, dres) in ((wq3, QT, hp, 'qt'), (wk3, KT, 2 + hp, 'kt')):
                for blk in range(4):
                    bank = pr.next()
                    for kc in range(8):
                        op('pe', lambda e: e.matmul(psb(bank), lhsT=w3[:, kc, :], rhs=u3[:, kc, blk * 512:(blk + 1) * 512],
                                                    start=(kc == 0), stop=(kc == 7)),
                           R=[('wbf', bqk), ures[blk]], W=[('ps', bank)], sig=(kc == 7))
                    op('act', lambda e: e.copy(out=RAW[:, 3 + blk * 512:3 + (blk + 1) * 512], in_=psb(bank)), R=[('ps', bank)], W=['raw'])
                cw = V_MCW + (l * 4 + chunk) * 4
                op('act', lambda e: e.activation(out=ACC, in_=RAW[:, 3:3 + S], func=AF.Identity, scale=vcol(cw + 3)), R=['raw', 'vecs'], W=['sq'])
                for tap in range(3):
                    op('dve', lambda e: e.scalar_tensor_tensor(out=ACC, in0=RAW[:, tap:tap + S], scalar=vcol(cw + tap), in1=ACC,
                                                               op0=ALU.mult, op1=ALU.add), R=['raw', 'vecs'], W=['sq'])
                op('act', lambda e: e.activation(out=dstT, in_=ACC, func=AF.Silu), R=['sq'], W=[dres])
            for t in range(16):
                if KTOK_DMA:
                    dma(KTOK3[:, t, :], KT[:, t * 128:(t + 1) * 128], R=['kt'], W=['ktok'], key='ktok', transpose=True)
                else:
                    tb = 7
                    pbv = PS[:, tb * 512:tb * 512 + 64].bitcast(BF16)
                    op('pe', lambda e: e.transpose(out=pbv, in_=KT[:, t * 128:(t + 1) * 128], identity=identb), R=['kt', 'cb'], W=[('ps', tb)])
                    op('dve', lambda e: e.tensor_copy(out=KTOK3[:, t, :], in_=pbv), R=[('ps', tb)], W=['ktok'])
            op('pool', lambda e: e.tensor_copy(out=DSEL[0:64, :], in_=DEC3[0:64, :, 2 * hp]), R=['dec'], W=['dsel'])
            op('pool', lambda e: e.tensor_copy(out=DSEL[64:128, :], in_=DEC3[64:128, :, 2 * hp + 1]), R=['dec'], W=['dsel'])
            op('pool', lambda e: e.memset(RF, 0.0), W=['rf'])
            gmb = bass.AP(vecs, V_MNG + l * 128, [[NV, 128], [0, 2], [1, 128]])

            VPA4 = RAW[:, 0:2080].bitcast(BF16).rearrange("p (t j c) -> p t j c", j=2, c=130)
            OGA3 = sq[:, :].rearrange("p (t c) -> p t c", c=256)
            gmb = bass.AP(vecs, V_MNG + l * 128, [[NV, 128], [0, 2], [1, 128]])
            for c in range(16):
                tk = slice(c * 128, (c + 1) * 128)
                bv_, bo_ = (0, 1) if c % 2 == 0 else (2, 3)
                for kc in range(8):
                    op('pe', lambda e: e.matmul(psb(bv_, 0, 256), lhsT=u3[:, kc, tk], rhs=wv3[:, kc, :], start=(kc == 0), stop=(kc == 7)),
                       R=[('wbf', bvv), ures[c // 4]], W=[('ps', bv_)], sig=(kc == 7))
                omb = bass.AP(OM.tensor, OM.offset + c * 4 + 2 * hp, [[ARENA, 128], [1, 2], [0, 128]])
                op('dve', lambda e: e.tensor_tensor(out=VPA4[:, c, :, 0:128], in0=psb(bv_, 0, 256).rearrange("p (j c) -> p j c", c=128), in1=omb, op=ALU.mult),
                   R=[('ps', bv_), 'om', 'raw', 'kt', 'qt'], W=[('vpa', c)])
                op('dve', lambda e: e.tensor_copy(out=VPA4[:, c, :, 128:129], in_=OM3[:, c, 2 * hp:2 * hp + 2].unsqueeze(2)), R=['om', 'raw'], W=[('vpa', c)])
                for kc in range(8):
                    op('pe', lambda e: e.matmul(psb(bo_, 0, 256), lhsT=u3[:, kc, tk], rhs=wo3[:, kc, :], start=(kc == 0), stop=(kc == 7)),
                       R=[('wbf', boo), ures[c // 4]], W=[('ps', bo_)], sig=(kc == 7))
                og_ = OGA3[:, c, :]
                op('act', lambda e: e.activation(out=og_, in_=psb(bo_, 0, 256), func=AF.Sigmoid), R=[('ps', bo_), 'sq', 'kt', 'qt'], W=[('oga', c)])
                op('pool', lambda e: e.tensor_tensor(out=og_.rearrange("p (j c) -> p j c", c=128), in0=og_.rearrange("p (j c) -> p j c", c=128), in1=gmb, op=ALU.mult),
                   R=['vecs'], W=[('oga', c)])

            def stage_a(c):
                tk = slice(c * 128, (c + 1) * 128)
                vi = c % 2
                for j in range(2):
                    pj = slice(j * 64, (j + 1) * 64)
                    sbk = 2 + j
                    op('pe', lambda e: e.matmul(psb(sbk, 0, 128), lhsT=KT[pj, tk], rhs=QT[pj, tk], start=True, stop=True),
                       R=['kt', 'qt'], W=[('ps', sbk)])
                    si_ = vi * 2 + j
                    op('dve', lambda e: e.tensor_tensor(out=STM[si_], in0=psb(sbk, 0, 128), in1=tri, op=ALU.mult),
                       R=[('ps', sbk), 'consts'], W=[('stm', si_)])

            def stage_s(c):
                vi = c % 2
                vp3 = VPA4[:, c]
                for j in range(2):
                    pj = slice(j * 64, (j + 1) * 64)
                    op('pe', lambda e: e.matmul(PS[pj, 512 + 256:512 + 385], lhsT=KTOK3[:, c, j * 64:(j + 1) * 64], rhs=vp3[:, j, 0:129], start=True, stop=True),
                       R=['ktok', ('vpa', c)], W=[('ps', 1)], sig=(j == 1))
                op('dve', lambda e: e.tensor_tensor(out=RF[:, 0:129], in0=psb(1, 256, 385), in1=RF[:, 0:129], op=ALU.add), R=[('ps', 1)], W=['rf'])
                op('dve', lambda e: e.tensor_scalar(out=RF[:, 0:129], in0=RF[:, 0:129], scalar1=DSEL[:, c:c + 1], scalar2=None, op0=ALU.mult),
                   R=['dsel'], W=['rf'])
                op('act', lambda e: e.copy(out=RBA3[:, c + 1, 0:129], in_=RF[:, 0:129]), R=['rf'], W=[('rba', c + 1)])

            def stage_b(c):
                tk = slice(c * 128, (c + 1) * 128)
                vi = c % 2
                vp3 = VPA4[:, c]
                og3 = OGA3[:, c, :].rearrange("p (j c) -> p j c", c=128)
                nbase = 4 + 2 * (c % 2)
                for j in range(2):
                    pj = slice(j * 64, (j + 1) * 64)
                    si_ = vi * 2 + j
                    nb_ = nbase + j
                    op('pe', lambda e: e.matmul(psb(nb_, 0, 129), lhsT=STM[si_], rhs=vp3[:, j, 0:129], start=True, stop=(c == 0)),
                       R=[('stm', si_), ('vpa', c)], W=[('ps', nb_)], sig=(c == 0))
                    if c > 0:
                        op('pe', lambda e: e.matmul(psb(nb_, 0, 129), lhsT=QT[pj, tk], rhs=RBA3[pj, c, 0:129], start=False, stop=True),
                           R=['qt', ('rba', c)], W=[('ps', nb_)])
                k0 = sm_ring.next() * 8
                r = sm[:, k0:k0 + 8]
                rres = ('smr', k0)
                den2 = bass.AP(PS, nbase * 512 + 128, [[4096, 128], [512, 2]])
                psi2 = PSI3[:, c, 2 * hp:2 * hp + 2]
                nres = [('ps', nbase), ('ps', nbase + 1)]
                op('dve', lambda e: e.tensor_tensor(out=r[:, 0:2], in0=den2, in1=psi2, op=ALU.mult), R=nres + ['psi'], W=[rres])
                op('dve', lambda e: e.scalar_tensor_tensor(out=r[:, 2:4], in0=r[:, 0:2], scalar=-1.0, in1=r[:, 0:2], op0=ALU.mult, op1=ALU.max), R=[rres], W=[rres])
                op('dve', lambda e: e.tensor_scalar(out=r[:, 2:4], in0=r[:, 2:4], scalar1=8.0, scalar2=None, op0=ALU.max), R=[rres], W=[rres])
                op('dve', lambda e: e.reciprocal(out=r[:, 4:6], in_=r[:, 2:4]), R=[rres], W=[rres])
                op('dve', lambda e: e.tensor_tensor(out=r[:, 4:6], in0=r[:, 4:6], in1=psi2, op=ALU.mult), R=['psi'], W=[rres])
                for j in range(2):
                    nb_ = nbase + j
                    op('act', lambda e: e.activation(out=HRs[j], in_=psb(nb_, 0, 128), func=AF.Identity, scale=r[:, 4 + j:5 + j]), R=[('ps', nb_), rres], W=[('hr', j)])
                    op('act', lambda e: e.activation(out=JK, in_=HRs[j], func=AF.Square, accum_out=r[:, 6 + j:7 + j]), R=[('hr', j)], W=['jk', (rres, 6 + j)])
                tiny_rstd(r[:, 6:8], r[:, 6:8], 1.0 / 128.0, EPS, [(rres, 6), (rres, 7)], [(rres, 'rs')], nh=nhalf2)
                for j in range(2):
                    hd = 2 * hp + j
                    yi = yt_ring.next()
                    op('dve', lambda e: e.scalar_tensor_tensor(out=YTm[yi], in0=HRs[j], scalar=r[:, 6 + j:7 + j], in1=og3[:, j, :], op0=ALU.mult, op1=ALU.mult),
                       R=[('hr', j), (rres, 'rs'), ('oga', c)], W=[('ytm', yi)])
                    dma(mix3[:, hd, tk], YTm[yi], R=[('ytm', yi)], W=[('mix', hd, c)], key=('ytmd', yi), transpose=True)

            stage_a(0)
            stage_s(0)
            for c in range(16):
                if c + 1 < 16:
                    stage_a(c + 1)
                    if c + 1 < 15:
                        stage_s(c + 1)
                stage_b(c)
        if dbg == 'mixB':
            return mix3
        wout_partial(l, 0, mix3)
        return None

    def ffn(l, s):
        AR.reset()
        U = AR.bf16(8 * 1024)
        U3 = U.rearrange("p (c t) -> p c t", t=1024)
        ACTT = AR.bf16(22 * 1024)
        A3 = ACTT.rearrange("p (c t) -> p c t", t=1024)
        G = [AR.f32(1024) for _ in range(2)]
        GG = [AR.bf16(1024) for _ in range(2)]
        GV = [AR.f32(1024) for _ in range(2)]
        HB = AR.f32(96)
        HB3 = HB.rearrange("p (c t) -> p c t", t=2)
        op('pool', lambda e: e.memset(HB, 0.0), W=['hb'])
        for half in range(2):
            norm(V_GFFN + l * 8, half * 1024, 1024, U3, 'uf')
            specs = []
            for cp in range(22):
                specs.append(([(0, 8, 128, w_up_d[l, :, cp * 128:(cp + 1) * 128]),
                               (1024, 8, 128, w_up_d[l, :, DFF + cp * 128:DFF + (cp + 1) * 128])], 2048))
            for oc in range(8):
                specs.append(([(0, 11, 128, w_dn_d[l, 0:1408, oc * 128:(oc + 1) * 128])], 1408))
                specs.append(([(0, 11, 128, w_dn_d[l, 1408:2816, oc * 128:(oc + 1) * 128])], 1408))
            ws = WStream(specs, look=2, eng='act')
            for cp in range(22):
                bi = ws.get(cp)
                gi_ = cp % 2
                for gv in range(2):
                    ch = cp + 22 * gv
                    b0 = 3 * gv
                    w3 = wv(bi, 1024 * gv, 8, 128)
                    op('act', lambda e: e.copy(out=psb(b0, 510, 512), in_=HB3[:, ch, :]), R=['hb'], W=[('ps', b0)])
                    for tb in range(2):
                        for kc in range(8):
                            op('pe', lambda e: e.matmul(psb(b0 + 1 + tb), lhsT=w3[:, kc, :], rhs=U3[:, kc, tb * 512:(tb + 1) * 512],
                                                        start=(kc == 0), stop=(kc == 7)),
                               R=[('wbf', bi), ('uf', tb)], W=[('ps', b0 + 1 + tb)], sig=(kc == 7))
                    if half == 0:
                        op('act', lambda e: e.copy(out=HB3[:, ch, :], in_=psb(b0 + 2, 510, 512)), R=[('ps', b0 + 2)], W=['hb'])
                    X0 = b0 * 512 + 510
                    pres = [('ps', b0), ('ps', b0 + 1), ('ps', b0 + 2)]
                    cw = V_FCW + (l * 44 + ch) * 3
                    cbias = V_FCB + l * 44 + ch
                    dst = G[gi_] if gv == 0 else GV[gi_]
                    dres = ('g', gi_) if gv == 0 else ('gv', gi_)
                    op('act', lambda e: e.activation(out=dst, in_=PS[:, X0 + 2:X0 + 1026], func=AF.Identity, scale=vcol(cw + 2), bias=vcol(cbias)),
                       R=pres + ['vecs'], W=[dres])
                    op('dve', lambda e: e.scalar_tensor_tensor(out=dst, in0=PS[:, X0 + 1:X0 + 1025], scalar=vcol(cw + 1), in1=dst, op0=ALU.mult, op1=ALU.add),
                       R=pres + ['vecs'], W=[dres])
                    op('dve', lambda e: e.scalar_tensor_tensor(out=dst, in0=PS[:, X0:X0 + 1024], scalar=vcol(cw), in1=dst, op0=ALU.mult, op1=ALU.add),
                       R=pres + ['vecs'], W=[dres])
                    if gv == 0:
                        op('act', lambda e: e.activation(out=GG[gi_], in_=dst, func=AF.Gelu), R=[dres], W=[('gg', gi_)])
                    else:
                        op('pool', lambda e: e.tensor_tensor(out=A3[:, cp, :], in0=GG[gi_], in1=dst, op=ALU.mult),
                           R=[('gg', gi_), dres], W=[('a', cp)])
            ring = Ring([6, 7])
            ares = [('a', cp) for cp in range(22)]
            for oc in range(8):
                b1 = ws.get(22 + 2 * oc)
                b2 = ws.get(22 + 2 * oc + 1)
                wd = [wv(b1, 0, 11, 128), wv(b2, 0, 11, 128)]
                for tb in range(2):
                    bank = ring.next()
                    for kc in range(22):
                        op('pe', lambda e: e.matmul(psb(bank), lhsT=wd[kc // 11][:, kc % 11, :], rhs=A3[:, kc, tb * 512:(tb + 1) * 512],
                                                    start=(kc == 0), stop=(kc == 21)),
                           R=[('wbf', b1), ('wbf', b2), ares[kc]], W=[('ps', bank)], sig=(kc == 21))
                    blk = half * 2 + tb
                    dst = hT3[:, oc, blk * 512:(blk + 1) * 512]
                    op('dve', lambda e: e.tensor_tensor(out=dst, in0=psb(bank), in1=dst, op=ALU.add), R=[('ps', bank)], W=[('h', blk)])

    def ple(l, s):
        AR.reset()
        uT = AR.bf16(8 * S)
        u3 = uT.rearrange("p (c t) -> p c t", t=S)
        PTt = AR.bf16(2 * S)
        PT3 = PTt.rearrange("p (c t) -> p c t", t=S)
        pst = [AR.f32(256), AR.f32(256)]
        SG = [AR.f32(512), AR.f32(512)]
        TT = [AR.f32(512), AR.f32(512)]
        norm(V_GPLE + l * 8, 0, S, u3, 'u')
        for t in range(16):
            b = t % 2
            dma(pst[b], p_d[l, s, t * 128:(t + 1) * 128, :], W=[('pst', b)])
            bank = psX.next()
            for c in range(2):
                op('pe', lambda e: e.transpose(out=psb(bank, c * 128, c * 128 + 128), in_=pst[b][:, c * 128:(c + 1) * 128], identity=identf),
                   R=[('pst', b), 'consts'], W=[('ps', bank)], sig=(c == 1))
            op('act', lambda e: e.copy(out=PT3[:, :, t * 128:(t + 1) * 128], in_=psb(bank, 0, 256).rearrange("p (c t) -> p c t", t=128)),
               R=[('ps', bank)], W=['ptt'])
        WP = AR.bf16(2048)
        loadw([(0, 2, 1024, w_pp_d[l, :, :])], 2048, dst_ap=WP, dst_res='wp')
        wp3 = WP.rearrange("p (k c) -> p k c", c=1024)
        specs = []
        for o2 in range(4):
            specs.append(([(0, 8, 256, w_pg_d[l, :, o2 * 256:(o2 + 1) * 256])], 2048))
        ws = WStream(specs, look=2)
        ring = Ring([4, 5, 6, 7])
        k = 0
        for o2 in range(4):
            bg_ = ws.get(o2)
            wg3 = wv(bg_, 0, 8, 256)
            for o1 in range(2):
                oc = o2 * 2 + o1
                for blk in range(4):
                    ba = ring.next()
                    bb = ring.next()
                    for kc in range(8):
                        op('pe', lambda e: e.matmul(psb(ba), lhsT=wg3[:, kc, o1 * 128:(o1 + 1) * 128], rhs=u3[:, kc, blk * 512:(blk + 1) * 512],
                                                    start=(kc == 0), stop=(kc == 7)),
                           R=[('wbf', bg_), ('u', blk)], W=[('ps', ba)], sig=(kc == 7))
                    for kc in range(2):
                        op('pe', lambda e: e.matmul(psb(bb), lhsT=wp3[:, kc, oc * 128:(oc + 1) * 128], rhs=PT3[:, kc, blk * 512:(blk + 1) * 512],
                                                    start=(kc == 0), stop=(kc == 1)),
                           R=['wp', 'ptt'], W=[('ps', bb)], sig=(kc == 1))
                    i = k % 2
                    k += 1
                    op('act', lambda e: e.activation(out=SG[i], in_=psb(ba), func=AF.Sigmoid), R=[('ps', ba)], W=[('sg', i)])
                    op('dve', lambda e: e.tensor_tensor(out=TT[i], in0=psb(bb), in1=SG[i], op=ALU.mult), R=[('ps', bb), ('sg', i)], W=[('tt', i)])
                    dst = hT3[:, oc, blk * 512:(blk + 1) * 512]
                    op('pool', lambda e: e.tensor_tensor(out=dst, in0=dst, in1=TT[i], op=ALU.add), R=[('tt', i)], W=[('h', blk)])

    def dump(ap3_or_none):
        sc.barrier()
        if ap3_or_none is None:
            dma(dbg_d[:, :], hT[:, :], R=[('h', b) for b in range(4)], key='dbg')
        else:
            n = ap3_or_none.shape[1] * ap3_or_none.shape[2]
            tmpf = arena[:, ARENA - 4 * S:ARENA]
            op('dve', lambda e: e.tensor_copy(out=tmpf[:, 0:n], in_=ap3_or_none.rearrange("p c t -> p (c t)")), W=['dbgt'])
            dma(dbg_d[:, 0:n], tmpf[:, 0:n], R=['dbgt'], key='dbg')
        sc.finish()

    for s in range(nseq):
        sc.barrier()
        phase_load(s)
        if dbg == 'x':
            dump(None)
            return nc
        for l in range(nlayer):
            sc.barrier()
            r = mixer(l, s)
            if r == 'stop':
                dump(None)
                return nc
            if dbg in ('mixA', 'mixB'):
                dump(r)
                return nc
            if dbg == 'mix':
                dump(None)
                return nc
            sc.barrier()
            ffn(l, s)
            if dbg == 'ffn':
                dump(None)
                return nc
            sc.barrier()
            ple(l, s)
            if dbg == 'ple':
                dump(None)
                return nc
        sc.barrier()
        phase_store(s)
    sc.finish()
    return nc


def _bucket_table():
    n = np.arange(4096)
    max_exact = 16
    nf = np.maximum(n, 1).astype(np.float32)
    large = max_exact + (np.log(nf / np.float32(max_exact)) / np.float32(math.log(128 / max_exact)) * np.float32(16)).astype(np.int32)
    large = np.minimum(large, 31)
    return np.where(n < max_exact, n, large)


def make_consts():
    c = np.zeros((128, NCONST), np.float32)
    c[:, K_ID:K_ID + 128] = np.eye(128, dtype=np.float32)
    i = np.arange(128)
    c[:, K_TRI:K_TRI + 128] = (i[:, None] <= i[None, :]).astype(np.float32)
    c[:, K_MNEG:K_MNEG + 128] = np.where(i[:, None] <= i[None, :], 0.0, NEG).astype(np.float32)
    bk = _bucket_table()
    lo = np.full(32, 1e9, np.float32)
    for b in range(31, -1, -1):
        idx = np.nonzero(bk == b)[0]
        lo[b] = idx[0] if len(idx) else (lo[b + 1] if b < 31 else 1e9)
    hi = np.concatenate([lo[1:], np.array([1e9], np.float32)])
    lo[0] = -1e9
    c[0:32, K_THR] = lo
    c[0:32, K_THR + 1] = hi
    return c


def make_vecs(inp):
    v = np.zeros((128, NV), np.float32)

    def fm(vec):
        return np.ascontiguousarray(vec.reshape(-1, 128).T)

    for l in range(DEPTH):
        v[:, V_GMIX + l * 8:V_GMIX + l * 8 + 8] = fm(inp['ln_mix_g'][l])
        v[:, V_GFFN + l * 8:V_GFFN + l * 8 + 8] = fm(inp['ln_ffn_g'][l])
        v[:, V_GPLE + l * 8:V_GPLE + l * 8 + 8] = fm(inp['ln_ple_g'][l])
        cw = inp['ffn_conv_w'][l]
        for tap in range(3):
            t_ = fm(cw[tap])
            v[:, V_FCW + l * 132 + tap:V_FCW + (l + 1) * 132:3] = t_
        v[:, V_FCB + l * 44:V_FCB + (l + 1) * 44] = fm(inp['ffn_conv_b'][l])
        mw = inp['mlstm_conv_w'][l]
        for tap in range(4):
            t_ = fm(mw[tap])
            v[:, V_MCW + l * 16 + tap:V_MCW + (l + 1) * 16:4] = t_
        v[:, V_GQ + l] = np.tile(inp['q_norm_g'][l], 2)
        v[:, V_GK + l] = np.tile(inp['k_norm_g'][l], 2)
        v[:, V_MNG + l * 128:V_MNG + (l + 1) * 128] = np.broadcast_to(inp['mlstm_norm_g'][l][None, :], (128, 128))
        v[:, V_SLG + l * 128:V_SLG + (l + 1) * 128] = np.broadcast_to(inp['diff_subln_g'][l][None, :], (128, 128))
        v[:, V_BI + l * 4:V_BI + (l + 1) * 4] = np.broadcast_to(inp['b_igate'][l][None, :], (128, 4))
        v[:, V_BF + l * 4:V_BF + (l + 1) * 4] = np.broadcast_to(inp['b_fgate'][l][None, :], (128, 4))
        for k, name in enumerate(('lam_q1', 'lam_k1', 'lam_q2', 'lam_k2')):
            v[:, V_LAM + l * 256 + k * 64:V_LAM + l * 256 + (k + 1) * 64] = np.broadcast_to(inp[name][l][None, :], (128, 64))
    v[0:32, V_RB:V_RB + 8] = inp['rel_bias'].reshape(32, 8)
    return v


def make_in_maps(inp, ncore=NCORE):
    f = lambda a: np.ascontiguousarray(np.asarray(a, dtype=np.float32))
    consts = make_consts()
    vecs = make_vecs({k: np.asarray(v) for k, v in inp.items()})
    shared = {
        'w_in': f(inp['w_in']), 'w_out': f(inp['w_out']), 'w_up': f(inp['w_up']), 'w_down': f(inp['w_down']),
        'w_ple_gate': f(inp['w_ple_gate']), 'w_ple_proj': f(inp['w_ple_proj']), 'consts': consts, 'vecs': vecs,
        'positions': np.ascontiguousarray(np.asarray(inp['positions'], dtype=np.int32)),
    }
    x = f(inp['x'])
    p = f(inp['p'])
    maps = []
    for c in range(ncore):
        m = dict(shared)
        m['x'] = np.ascontiguousarray(x[2 * c:2 * c + 2])
        m['p'] = np.ascontiguousarray(p[:, 2 * c:2 * c + 2])
        maps.append(m)
    return maps


def kernel(**inputs):
    nc = build()
    maps = make_in_maps(inputs)
    res = run_bass_kernel_spmd(nc, maps, core_ids=list(range(NCORE)))
    out = np.concatenate([np.asarray(r['out']) for r in res.results], axis=0)
    return out.astype(np.float32)
```

```python
import math
import numpy as np
import concourse.bass as bass
import concourse.mybir as mybir
from concourse.bass_utils import run_bass_kernel_spmd

F32 = mybir.dt.float32
BF16 = mybir.dt.bfloat16
I32 = mybir.dt.int32
ALU = mybir.AluOpType
AF = mybir.ActivationFunctionType

D = 1024
S = 2048
DEPTH = 2
NCORE = 8
IN_COLS = 3080
C_QM, C_KM, C_VM, C_OM, C_IM, C_FM, C_QD, C_KD, C_VD = 0, 256, 512, 1024, 1536, 1540, 1544, 2056, 2568
DFF = 2816
EPS = 1e-6
NEG = -30000.0

K_ID, K_TRI, K_MNEG, K_OH = 0, 128, 256, 384
K_THR = 768
NCONST = 770
V_GMIX, V_GFFN, V_GPLE = 0, 16, 32
V_FCW = 48
V_FCB = V_FCW + 264
V_MCW = V_FCB + 88
V_GQ = V_MCW + 32
V_GK = V_GQ + 2
V_MNG = V_GK + 2
V_SLG = V_MNG + 256
V_BI = V_SLG + 256
V_BF = V_BI + 8
V_LAM = V_BF + 8
V_RB = V_LAM + 512
NV = V_RB + 8


SAME_ENGINE_INORDER = ('pe',)
KTOK_DMA = False


class Sched:
    def __init__(self, nc):
        self.nc = nc
        self.E = {'pe': nc.tensor, 'act': nc.scalar, 'dve': nc.vector, 'pool': nc.gpsimd, 'sp': nc.sync}
        self.sem = {e: nc.alloc_semaphore('s_' + e) for e in self.E}
        self.cnt = {e: 0 for e in self.E}
        self.waited = {e: {} for e in self.E}
        self.lastw = {}
        self.readers = {}
        self.dsem = {}
        self.dcnt = {}
        self.nins = 0

    def _wait(self, e, tok):
        kind, key, val = tok
        if kind == 'eng' and key == e and e in SAME_ENGINE_INORDER:
            return
        w = self.waited[e]
        k = (kind, key)
        if w.get(k, 0) >= val:
            return
        sem = self.sem[key] if kind == 'eng' else self.dsem[key]
        self.E[e].wait_ge(sem, val)
        w[k] = val

    def _sync(self, e, R, W):
        for r in R:
            t = self.lastw.get(r)
            if t:
                self._wait(e, t)
        for w in W:
            t = self.lastw.get(w)
            if t:
                self._wait(e, t)
            rd = self.readers.get(w)
            if rd:
                for (kind, key), val in rd.items():
                    self._wait(e, (kind, key, val))

    def _record(self, tok, R, W):
        for w in W:
            self.lastw[w] = tok
            self.readers[w] = {}
        k = (tok[0], tok[1])
        for r in R:
            d = self.readers.setdefault(r, {})
            if d.get(k, 0) < tok[2]:
                d[k] = tok[2]

    def op(self, e, fn, R=(), W=(), sig=True):
        self._sync(e, R, W)
        ins = fn(self.E[e])
        self.nins += 1
        if sig:
            self.cnt[e] += 1
            ins.then_inc(self.sem[e], 1)
            tok = ('eng', e, self.cnt[e])
        else:
            tok = ('eng', e, self.cnt[e] + 1)
        self._record(tok, R, W)
        return ins

    def dma(self, out, in_, R=(), W=(), key=None, q='sp', transpose=False):
        if key is None:
            key = W[0] if W else R[0]
        if key not in self.dsem:
            self.dsem[key] = self.nc.alloc_semaphore('d%d' % len(self.dsem))
            self.dcnt[key] = 0
        self._sync(q, R, W)
        self.dcnt[key] += 16
        if transpose:
            self.E[q].dma_start_transpose(out=out, in_=in_).then_inc(self.dsem[key], 16)
        else:
            self.E[q].dma_start(out=out, in_=in_).then_inc(self.dsem[key], 16)
        self.nins += 1
        self._record(('dma', key, self.dcnt[key]), R, W)

    def barrier(self):
        for e in self.E:
            for e2 in self.E:
                if e2 != e and self.cnt[e2] > 0:
                    self._wait(e, ('eng', e2, self.cnt[e2]))
            for k in self.dsem:
                self._wait(e, ('dma', k, self.dcnt[k]))

    def finish(self):
        for k in self.dsem:
            self._wait('sp', ('dma', k, self.dcnt[k]))
        for e2 in self.E:
            if e2 != 'sp' and self.cnt[e2] > 0:
                self._wait('sp', ('eng', e2, self.cnt[e2]))


class Ring:
    def __init__(self, items):
        self.items = list(items)
        self.i = 0

    def next(self):
        v = self.items[self.i % len(self.items)]
        self.i += 1
        return v


def build(nseq=2, nlayer=DEPTH, dbg=None):
    nc = bass.Bass("TRN2", target_bir_lowering=False)
    x_d = nc.dram_tensor("x", [2, S, D], F32, kind="ExternalInput")
    p_d = nc.dram_tensor("p", [DEPTH, 2, S, 256], F32, kind="ExternalInput")
    w_in_d = nc.dram_tensor("w_in", [DEPTH, D, IN_COLS], F32, kind="ExternalInput")
    w_out_d = nc.dram_tensor("w_out", [DEPTH, D, D], F32, kind="ExternalInput")
    w_up_d = nc.dram_tensor("w_up", [DEPTH, D, 2 * DFF], F32, kind="ExternalInput")
    w_dn_d = nc.dram_tensor("w_down", [DEPTH, DFF, D], F32, kind="ExternalInput")
    w_pg_d = nc.dram_tensor("w_ple_gate", [DEPTH, D, D], F32, kind="ExternalInput")
    w_pp_d = nc.dram_tensor("w_ple_proj", [DEPTH, 256, D], F32, kind="ExternalInput")
    consts_d = nc.dram_tensor("consts", [128, NCONST], F32, kind="ExternalInput")
    pos_d = nc.dram_tensor("positions", [S], I32, kind="ExternalInput")
    vecs_d = nc.dram_tensor("vecs", [128, NV], F32, kind="ExternalInput")
    out_d = nc.dram_tensor("out", [2, S, D], F32, kind="ExternalOutput")
    scr_d = nc.dram_tensor("scr", [8, 128, 384], F32, kind="Internal")
    dbg_d = None
    if dbg is not None:
        dbg_d = nc.dram_tensor("dbg", [128, 8 * S], F32, kind="ExternalOutput")

    sc = Sched(nc)
    op, dma = sc.op, sc.dma

    hT = nc.alloc_sbuf_tensor("hT", [128, 8 * S], F32)
    hT3 = hT[:, :].rearrange("p (c t) -> p c t", t=S)
    consts = nc.alloc_sbuf_tensor("consts_sb", [128, NCONST], F32)
    vecs = nc.alloc_sbuf_tensor("vecs_sb", [128, NV], F32)
    biasT = nc.alloc_sbuf_tensor("biasT", [128, 16 * 128], F32)
    wst = [nc.alloc_sbuf_tensor("wst%d" % i, [128, 2048], F32) for i in range(2)]
    wbf = [nc.alloc_sbuf_tensor("wbf%d" % i, [128, 2048], BF16) for i in range(4)]
    cb = nc.alloc_sbuf_tensor("cb", [128, 3 * 128], BF16)
    ct = nc.alloc_sbuf_tensor("ct", [128, 16], F32)
    sq = nc.alloc_sbuf_tensor("sq", [128, 8 * 512], BF16)
    sd = nc.alloc_sbuf_tensor("sd", [128, 512], F32)
    rstd = nc.alloc_sbuf_tensor("rstd", [128, 512], F32)
    sm = nc.alloc_sbuf_tensor("sm", [128, 64], F32)
    ARENA = 20800
    arena = nc.alloc_sbuf_tensor("arena", [128, ARENA], F32)
    PS = nc.alloc_psum_tensor("ps", [128, 4096], F32)

    identf = consts[:, K_ID:K_ID + 128]
    tri = consts[:, K_TRI:K_TRI + 128]
    identb = cb[:, 0:128]
    onesb = cb[:, 128:256]
    blk1 = cb[:, 256:384]
    eps_c = ct[:, 0:1]
    one_c = ct[:, 1:2]
    nhalf_c = ct[:, 2:3]
    nhalf2 = ct[:, 8:10]

    class Arena:
        def __init__(self):
            self.off = 0

        def reset(self):
            self.off = 0

        def f32(self, n):
            a = arena[:, self.off:self.off + n]
            self.off += n
            assert self.off <= ARENA, self.off
            return a

        def bf16(self, n):
            assert n % 2 == 0
            return self.f32(n // 2).bitcast(BF16)

    AR = Arena()

    def psb(bank, a=0, b=512):
        return PS[:, bank * 512 + a: bank * 512 + b]

    def vcol(c, n=1):
        return vecs[:, c:c + n]

    wring = {'st': 0, 'bf': 0}

    def loadw(parts, n, dst_ap=None, dst_res=None, eng='pool'):
        si = wring['st'] % 2
        wring['st'] += 1
        for (off, nk, ncol, src) in parts:
            dst = wst[si][:, off:off + nk * ncol].rearrange("p (k c) -> p k c", c=ncol)
            dma(dst, src.rearrange("(k p) c -> p k c", p=128), W=[('wst', si)])
        if dst_ap is not None:
            op('pool', lambda e: e.tensor_copy(out=dst_ap[:, 0:n], in_=wst[si][:, 0:n]), R=[('wst', si)], W=[dst_res])
            return None
        bi = wring['bf'] % 4
        wring['bf'] += 1
        if eng == 'act':
            op('act', lambda e: e.copy(out=wbf[bi][:, 0:n], in_=wst[si][:, 0:n]), R=[('wst', si)], W=[('wbf', bi)])
        else:
            op(eng, lambda e: e.tensor_copy(out=wbf[bi][:, 0:n], in_=wst[si][:, 0:n]), R=[('wst', si)], W=[('wbf', bi)])
        return bi

    def wv(bi, off, nk, ncol):
        return wbf[bi][:, off:off + nk * ncol].rearrange("p (k c) -> p k c", c=ncol)

    class WStream:
        def __init__(self, specs, look=2, eng='pool'):
            self.specs = specs
            self.look = look
            self.eng = eng
            self.issued = []

        def get(self, i):
            while len(self.issued) < min(len(self.specs), i + 1 + self.look):
                parts, n = self.specs[len(self.issued)]
                self.issued.append(loadw(parts, n, eng=self.eng))
            return self.issued[i]

    dma(consts[:, :], consts_d[:, :], W=['consts'])
    dma(vecs[:, :], vecs_d[:, :], W=['vecs'])
    op('pool', lambda e: e.memset(ct[:, 0:1], EPS), W=['ct'])
    op('pool', lambda e: e.memset(ct[:, 1:2], 1.0), W=['ct'])
    op('pool', lambda e: e.memset(ct[:, 2:3], -0.5), W=['ct'])
    op('pool', lambda e: e.memset(ct[:, 3:4], 0.0), W=['ct'])
    op('pool', lambda e: e.memset(ct[:, 8:10], -0.5), W=['ct'])
    op('pool', lambda e: e.tensor_copy(out=identb, in_=identf), R=['consts'], W=['cb'])
    op('pool', lambda e: e.memset(onesb, 1.0), W=['cb'])
    op('pool', lambda e: e.memset(blk1, 0.0), W=['cb'])
    op('pool', lambda e: e.memset(cb[0:64, 256:320], 1.0), W=['cb'])
    op('pool', lambda e: e.memset(cb[64:128, 320:384], 1.0), W=['cb'])
    lam_init = [0.8 - 0.6 * math.exp(-0.3 * l) for l in range(DEPTH)]
    for l in range(DEPTH):
        base = V_LAM + l * 256
        op('dve', lambda e: e.scalar_tensor_tensor(out=sd[:, 0:64], in0=vcol(base, 64), scalar=1.0, in1=vcol(base + 64, 64),
                                                   op0=ALU.mult, op1=ALU.mult, accum_out=sm[:, 0:1]), R=['vecs'], W=['sd', 'sm'])
        op('dve', lambda e: e.scalar_tensor_tensor(out=sd[:, 0:64], in0=vcol(base + 128, 64), scalar=1.0, in1=vcol(base + 192, 64),
                                                   op0=ALU.mult, op1=ALU.mult, accum_out=sm[:, 1:2]), R=['vecs'], W=['sd', 'sm'])
        op('act', lambda e: e.activation(out=sm[:, 2:4], in_=sm[:, 0:2], func=AF.Exp), R=['sm'], W=['sm2'])
        op('dve', lambda e: e.tensor_tensor(out=sm[:, 4:5], in0=sm[:, 3:4], in1=sm[:, 2:3], op=ALU.subtract), R=['sm2'], W=['sm3'])
        op('dve', lambda e: e.tensor_scalar(out=ct[:, 4 + l:5 + l], in0=sm[:, 4:5], scalar1=-lam_init[l], scalar2=None, op0=ALU.add),
           R=['sm3'], W=['ct'])
        op('pool', lambda e: e.memset(ct[:, 6 + l:7 + l], EPS / (1.0 - lam_init[l]) ** 2), W=['ct'])
    posi = arena[0:32, 0:257].bitcast(I32)
    sqf = sq[:, :].bitcast(F32)
    dma(posi, bass.AP(pos_d, 0, [[0, 32], [1, 257]]), W=['posi'])
    op('pool', lambda e: e.memset(sd[0:32, 0:127], 0.0), W=['sd'])
    op('dve', lambda e: e.tensor_copy(out=sd[0:32, 127:384], in_=posi), R=['posi'], W=['sd'])
    op('dve', lambda e: e.tensor_copy(out=sm[0:32, 8:9], in_=sd[0:32, 127:128]), R=['sd'], W=['sm'])
    op('dve', lambda e: e.tensor_scalar(out=sd[0:32, 127:384], in0=sd[0:32, 127:384], scalar1=sm[0:32, 8:9], scalar2=None, op0=ALU.subtract),
       R=['sm'], W=['sd'])
    op('dve', lambda e: e.tensor_scalar(out=sqf[0:32, 0:384], in0=sd[0:32, 0:384], scalar1=consts[0:32, K_THR:K_THR + 1], scalar2=None, op0=ALU.is_ge),
       R=['sd', 'consts'], W=['sq'])
    op('dve', lambda e: e.tensor_scalar(out=sqf[0:32, 384:768], in0=sd[0:32, 0:384], scalar1=consts[0:32, K_THR + 1:K_THR + 2], scalar2=None, op0=ALU.is_lt),
       R=['sd', 'consts'], W=['sq'])
    op('dve', lambda e: e.tensor_tensor(out=sqf[0:32, 768:1152], in0=sqf[0:32, 0:384], in1=sqf[0:32, 384:768], op=ALU.mult), R=['sq'], W=['sq'])
    op('pe', lambda e: e.matmul(PS[0:8, 0:384], lhsT=vecs[0:32, V_RB:V_RB + 8], rhs=sqf[0:32, 768:1152], start=True, stop=True),
       R=['vecs', 'sq'], W=[('ps', 0)])
    op('act', lambda e: e.copy(out=sd[0:8, 0:1], in_=PS[0:8, 383:384]), R=[('ps', 0)], W=['sd'])
    op('dve', lambda e: e.tensor_scalar(out=rstd[0:8, 0:384], in0=PS[0:8, 0:384], scalar1=sd[0:8, 0:1], scalar2=None, op0=ALU.subtract),
       R=[('ps', 0), 'sd'], W=['rstd'])
    dma(scr_d[:, :, :], bass.AP(rstd, 0, [[512, 8], [0, 128], [1, 384]]), R=['rstd'], W=['scr'], key='scr')
    for dl in range(2):
        for hc in range(8):
            src = bass.AP(scr_d, hc * 128 * 384 + 127 + 128 * dl, [[383, 128], [1, 128]])
            dma(biasT[:, (dl * 8 + hc) * 128:(dl * 8 + hc + 1) * 128], src, R=['scr'], W=['biasT'], key='biasT')
    b0v = biasT[:, 0:1024].rearrange("p (h q) -> p h q", q=128)
    op('dve', lambda e: e.tensor_tensor(out=b0v, in0=b0v, in1=bass.AP(consts, K_MNEG, [[NCONST, 128], [0, 8], [1, 128]]), op=ALU.add),
       R=['biasT', 'consts'], W=['biasT'])

    op('act', lambda e: e.activation(out=biasT[:, :], in_=biasT[:, :], func=AF.Exp), R=['biasT'], W=['biasT'])

    psX = Ring([0, 1, 2, 3])

    def phase_load(s):
        AR.reset()
        xs = [AR.f32(1024), AR.f32(1024)]
        for t in range(16):
            b = t % 2
            dma(xs[b], x_d[s, t * 128:(t + 1) * 128, :], W=[('xs', b)])
            for half in range(2):
                bank = psX.next()
                for c4 in range(4):
                    c = half * 4 + c4
                    op('pe', lambda e: e.transpose(out=psb(bank, c4 * 128, c4 * 128 + 128), in_=xs[b][:, c * 128:(c + 1) * 128], identity=identf),
                       R=[('xs', b), 'consts'], W=[('ps', bank)], sig=(c4 == 3))
                eng = 'act' if half == 0 else 'dve'
                src = psb(bank).rearrange("p (c t) -> p c t", t=128)
                dst = hT3[:, half * 4:half * 4 + 4, t * 128:(t + 1) * 128]
                if eng == 'act':
                    op('act', lambda e: e.copy(out=dst, in_=src), R=[('ps', bank)], W=[('h', t // 4)])
                else:
                    op('dve', lambda e: e.tensor_copy(out=dst, in_=src), R=[('ps', bank)], W=[('h', t // 4)])

    def phase_store(s):
        AR.reset()
        os_ = [AR.f32(1024), AR.f32(1024)]
        for t in range(16):
            b = t % 2
            for half in range(2):
                bank = psX.next()
                for c4 in range(4):
                    c = half * 4 + c4
                    op('pe', lambda e: e.transpose(out=psb(bank, c4 * 128, c4 * 128 + 128), in_=hT3[:, c, t * 128:(t + 1) * 128], identity=identf),
                       R=[('h', t // 4), 'consts'], W=[('ps', bank)], sig=(c4 == 3))
                dst = os_[b][:, half * 512:(half + 1) * 512]
                if half == 0:
                    op('act', lambda e: e.copy(out=dst, in_=psb(bank)), R=[('ps', bank)], W=[('os', b)])
                else:
                    op('dve', lambda e: e.tensor_copy(out=dst, in_=psb(bank)), R=[('ps', bank)], W=[('os', b)])
            dma(out_d[s, t * 128:(t + 1) * 128, :], os_[b], R=[('os', b)], key=('os', b))

    psN = Ring([6, 7])

    def norm(gcol, tok0, ntok, u3, ures):
        sq3 = sq[:, :].rearrange("p (c t) -> p c t", t=512)
        for b in range(ntok // 512):
            t0 = tok0 + b * 512
            hres = ('h', t0 // 512)
            op('act', lambda e: e.activation(out=sq3, in_=hT3[:, :, t0:t0 + 512], func=AF.Square), R=[hres], W=['sq'])
            bank = psN.next()
            for c in range(8):
                op('pe', lambda e: e.matmul(psb(bank), lhsT=onesb, rhs=sq3[:, c, :], start=(c == 0), stop=(c == 7)),
                   R=['sq', 'cb'], W=[('ps', bank)], sig=(c == 7))
            op('act', lambda e: e.activation(out=sd[:, :], in_=psb(bank), func=AF.Ln, bias=eps_c, scale=1.0 / D),
               R=[('ps', bank), 'ct'], W=['sd'])
            op('act', lambda e: e.activation(out=rstd[:, :], in_=sd[:, :], func=AF.Exp, scale=-0.5), R=['sd'], W=['rstd'])
            for c in range(8):
                op('dve', lambda e: e.scalar_tensor_tensor(out=u3[:, c, b * 512:(b + 1) * 512], in0=hT3[:, c, t0:t0 + 512],
                                                           scalar=vcol(gcol + c), in1=rstd[:, :], op0=ALU.mult, op1=ALU.mult),
                   R=[hres, 'rstd', 'vecs'], W=[(ures, b)])

    def tiny_rstd(dst, src, scale, eps_f, Rr, Ww, nh=None):
        op('pool', lambda e: e.tensor_scalar(out=dst, in0=src, scalar1=scale, scalar2=eps_f, op0=ALU.mult, op1=ALU.add), R=Rr, W=Ww)
        op('pool', lambda e: e.tensor_tensor(out=dst, in0=dst, in1=(nhalf_c if nh is None else nh), op=ALU.pow), R=Ww, W=Ww)

    def wout_partial(l, r0, mix3):
        specs = []
        for half in range(2):
            src = w_out_d[l, r0:r0 + 512, half * 512:(half + 1) * 512]
            specs.append(([(0, 4, 512, src)], 2048))
        ws = WStream(specs)
        ring = Ring([4, 5, 6, 7])
        for half in range(2):
            bi = ws.get(half)
            w3 = wv(bi, 0, 4, 512)
            for o4 in range(4):
                oc = half * 4 + o4
                for blk in range(4):
                    bank = ring.next()
                    for kc in range(4):
                        op('pe', lambda e: e.matmul(psb(bank), lhsT=w3[:, kc, o4 * 128:(o4 + 1) * 128], rhs=mix3[:, kc, blk * 512:(blk + 1) * 512],
                                                    start=(kc == 0), stop=(kc == 3)),
                           R=[('wbf', bi)] + [('mix', kc, 4 * blk + t_) for t_ in range(4)], W=[('ps', bank)], sig=(kc == 3))
                    dst = hT3[:, oc, blk * 512:(blk + 1) * 512]
                    op('dve', lambda e: e.tensor_tensor(out=dst, in0=psb(bank), in1=dst, op=ALU.add), R=[('ps', bank)], W=[('h', blk)])

    def mixer(l, s):
        AR.reset()
        uT = AR.bf16(8 * S)
        u3 = uT.rearrange("p (c t) -> p c t", t=S)
        mixT = AR.bf16(4 * S)
        mix3 = mixT.rearrange("p (c t) -> p c t", t=S)
        stage_off = AR.off
        specs = []
        for h in range(4):
            specs.append(([(0, 8, 128, w_in_d[l, :, C_QD + h * 128:C_QD + (h + 1) * 128]),
                           (1024, 8, 128, w_in_d[l, :, C_KD + h * 128:C_KD + (h + 1) * 128])], 2048))
            specs.append(([(0, 8, 128, w_in_d[l, :, C_VD + h * 128:C_VD + (h + 1) * 128])], 1024))
        ws = WStream(specs, look=2)
        ws.get(0)
        ws.get(1)
        norm(V_GMIX + l * 8, 0, S, u3, 'u')
        ures = [('u', b) for b in range(4)]
        if dbg == 'm1':
            return 'stop'

        QTs = [AR.bf16(S) for _ in range(2)]
        KTs = [AR.bf16(S) for _ in range(2)]
        VPs = [AR.bf16(16 * 130) for _ in range(2)]
        VP3s = [v_.rearrange("p (t c) -> p t c", c=130) for v_ in VPs]
        PT = [AR.bf16(1024) for _ in range(2)]
        TMP = [sd[:, :], rstd[:, :], PT[0].bitcast(F32), PT[1].bitcast(F32)]
        sdres = [['sd'], ['rstd'], [('pt', 0, 0), ('pt', 0, 1)], [('pt', 1, 0), ('pt', 1, 1)]]
        JK = sq[:, 2048:2176]
        YT = [AR.bf16(128) for _ in range(2)]
        ACCS = [AR.f32(258) for _ in range(4)]
        o_ring = Ring([0, 1, 2, 3])
        for hb in range(2):
            op('pool', lambda e: e.memset(VP3s[hb][:, :, 128:130], 1.0), W=[('vp1', hb)])
        scr_ring = Ring([(4, 6), (5, 7)])
        prj_ring = Ring([(0, 1), (2, 3), (4, 6), (5, 7)])
        pt_ring = Ring([0, 1])
        qn_ring = Ring([0, 1, 2, 3])
        yt_ring = Ring([0, 1])
        sm_ring = Ring([0, 1, 2, 3])
        sc_att = 1.0 / 8.0
        sub_scale = 1.0 / (128.0 * (1.0 - lam_init[l]) ** 2)

        def proj_gen(h):
            hb = h % 2
            bqk = ws.get(2 * h)
            bv = ws.get(2 * h + 1)
            wq3 = wv(bqk, 0, 8, 128)
            wk3 = wv(bqk, 1024, 8, 128)
            wv3 = wv(bv, 0, 8, 128)
            for (w3, dstT, gcol, dres) in ((wq3, QTs[hb], V_GQ + l, 'qt'), (wk3, KTs[hb], V_GK + l, 'kt')):
                for blk in range(4):
                    bank, bank2 = prj_ring.next()
                    for kc in range(8):
                        op('pe', lambda e: e.matmul(psb(bank), lhsT=w3[:, kc, :], rhs=u3[:, kc, blk * 512:(blk + 1) * 512],
                                                    start=(kc == 0), stop=(kc == 7)),
                           R=[('wbf', bqk), ures[blk]], W=[('ps', bank)], sig=(kc == 7))
                    qi = qn_ring.next()
                    sqs = sq[:, qi * 512:(qi + 1) * 512]
                    sdq = TMP[qi]
                    op('act', lambda e: e.activation(out=sqs, in_=psb(bank), func=AF.Square), R=[('ps', bank)], W=[('sqs', qi)])
                    op('pe', lambda e: e.matmul(psb(bank2), lhsT=blk1, rhs=sqs, start=True, stop=True),
                       R=[('sqs', qi), 'cb'], W=[('ps', bank2)])
                    op('act', lambda e: e.activation(out=sdq, in_=psb(bank2), func=AF.Ln, bias=eps_c, scale=1.0 / 64.0),
                       R=[('ps', bank2), 'ct'], W=sdres[qi])
                    op('act', lambda e: e.activation(out=sdq, in_=sdq, func=AF.Exp, scale=-0.5), R=sdres[qi], W=sdres[qi])
                    op('dve', lambda e: e.scalar_tensor_tensor(out=dstT[:, blk * 512:(blk + 1) * 512], in0=psb(bank), scalar=vcol(gcol),
                                                               in1=sdq, op0=ALU.mult, op1=ALU.mult),
                       R=[('ps', bank), 'vecs'] + sdres[qi], W=[(dres, hb, blk)])
                    yield
            for t4 in range(4):
                bank, _ = prj_ring.next()
                for tt in range(4):
                    t = t4 * 4 + tt
                    for kc in range(8):
                        op('pe', lambda e: e.matmul(psb(bank, tt * 128, tt * 128 + 128), lhsT=u3[:, kc, t * 128:(t + 1) * 128], rhs=wv3[:, kc, :],
                                                    start=(kc == 0), stop=(kc == 7)),
                           R=[('wbf', bv), ures[t4]], W=[('ps', bank)], sig=(kc == 7 and tt == 3))
                op('act', lambda e: e.copy(out=VP3s[hb][:, t4 * 4:t4 * 4 + 4, 0:128], in_=psb(bank).rearrange("p (t c) -> p t c", c=128)),
                   R=[('ps', bank)], W=[('vp', hb, t4)])
                yield

        for h in range(4):
            hb = h % 2
            if hb == 0:
                for _ in proj_gen(h):
                    pass
                for _ in proj_gen(h + 1):
                    pass
            QT, KT, VP3 = QTs[hb], KTs[hb], VP3s[hb]
            gnext = None
            def emit_scores(qb, jp):
                sbs = scr_ring.next()
                for comp in range(2):
                    for jj in range(2):
                        j = 2 * jp + jj
                        op('pe', lambda e: e.matmul(psb(sbs[comp], jj * 256, jj * 256 + 256),
                                                    lhsT=KT[comp * 64:(comp + 1) * 64, j * 128:(j + 1) * 128],
                                                    rhs=QT[comp * 64:(comp + 1) * 64, qb * 256:qb * 256 + 256], start=True, stop=True),
                           R=[('kt', hb, j // 4), ('qt', hb, qb // 2)], W=[('ps', sbs[comp])], sig=(jj == 1))
                return sbs

            def emit_exp(qb, jp, sbs):
                pi = pt_ring.next()
                pt = PT[pi]
                active = []
                for comp in range(2):
                    op('act', lambda e: e.activation(out=pt[:, comp * 512:comp * 512 + 512], in_=psb(sbs[comp]), func=AF.Exp, scale=sc_att),
                       R=[('ps', sbs[comp])], W=[('pt', pi, comp)])
                for jj in range(2):
                    j = 2 * jp + jj
                    for t in range(2):
                        dl = 2 * qb + t - j
                        if dl < 0:
                            continue
                        for comp in range(2):
                            if dl < 2:
                                a_ = comp * 512 + jj * 256 + t * 128
                                bt = biasT[:, (dl * 8 + h * 2 + comp) * 128:(dl * 8 + h * 2 + comp + 1) * 128]
                                op('dve', lambda e: e.tensor_tensor(out=pt[:, a_:a_ + 128], in0=pt[:, a_:a_ + 128], in1=bt, op=ALU.mult),
                                   R=['biasT'], W=[('pt', pi, comp)])
                            active.append((jj, comp, t))
                return (pi, active)

            def emit_pv(qb, jp, pi, active):
                pt = PT[pi]
                for (jj, comp, t) in active:
                    j = 2 * jp + jj
                    i = 2 * qb + t
                    ab = t * 2 + comp
                    a_ = comp * 512 + jj * 256 + t * 128
                    op('pe', lambda e: e.matmul(psb(ab, 0, 129), lhsT=pt[:, a_:a_ + 128],
                                                rhs=VP3[:, j, 0:129], start=(j == 0), stop=(j == i)),
                       R=[('pt', pi, comp), ('vp', hb, j // 4), ('vp1', hb)], W=[('ps', ab)], sig=True)

            def emit_finalize1(qb):
                out = []
                for t in range(2):
                    i = 2 * qb + t
                    a1, a2 = t * 2, t * 2 + 1
                    k0 = sm_ring.next() * 8
                    r = sm[:, k0:k0 + 8]
                    rres = ('smr', k0)
                    oi = o_ring.next()
                    ac = ACCS[oi]
                    op('dve', lambda e: e.tensor_copy(out=ac[:, 0:129], in_=psb(a1, 0, 129)), R=[('ps', a1)], W=[('accs', oi, 0)])
                    op('dve', lambda e: e.tensor_copy(out=ac[:, 129:258], in_=psb(a2, 0, 129)), R=[('ps', a2)], W=[('accs', oi, 1)])
                    op('dve', lambda e: e.reciprocal(out=r[:, 0:1], in_=ac[:, 128:129]), R=[('accs', oi, 0)], W=[rres])
                    op('dve', lambda e: e.reciprocal(out=r[:, 1:2], in_=ac[:, 257:258]), R=[('accs', oi, 1)], W=[rres])
                    op('dve', lambda e: e.tensor_tensor(out=r[:, 1:2], in0=r[:, 1:2], in1=ct[:, 4 + l:5 + l], op=ALU.mult), R=['ct'], W=[rres])
                    OOi = ac[:, 129:257]
                    op('dve', lambda e: e.tensor_scalar(out=ac[:, 0:128], in0=ac[:, 0:128], scalar1=r[:, 0:1], scalar2=None, op0=ALU.mult),
                       R=[rres], W=[('accs', oi, 0)])
                    op('dve', lambda e: e.scalar_tensor_tensor(out=OOi, in0=OOi, scalar=r[:, 1:2], in1=ac[:, 0:128], op0=ALU.mult, op1=ALU.add),
                       R=[('accs', oi, 0), rres], W=[('accs', oi, 1)])
                    op('dve', lambda e: e.scalar_tensor_tensor(out=ac[:, 0:128], in0=OOi, scalar=1.0, in1=OOi, op0=ALU.mult, op1=ALU.mult,
                                                               accum_out=r[:, 2:3]), R=[('accs', oi, 1)], W=[('accs', oi, 0), (rres, 2)])
                    tiny_rstd(r[:, 3:4], r[:, 2:3], sub_scale, EPS / (1.0 - lam_init[l]) ** 2, [(rres, 2), 'ct'], [(rres, 3)])
                    out.append((i, oi, r, rres))
                return out

            def emit_finalize2(items):
                for (i, oi, r, rres) in items:
                    OOi = ACCS[oi][:, 129:257]
                    yi = yt_ring.next()
                    op('dve', lambda e: e.scalar_tensor_tensor(out=YT[yi], in0=OOi, scalar=r[:, 3:4], in1=vcol(V_SLG + l * 128, 128),
                                                               op0=ALU.mult, op1=ALU.mult), R=[('accs', oi, 1), (rres, 3), 'vecs'], W=[('yt', yi)])
                    dma(mix3[:, h, i * 128:(i + 1) * 128], YT[yi], R=[('yt', yi)], W=[('mix', h, i)], key=('ytd', yi), transpose=True)

            steps = [(qb, jp) for qb in range(8) for jp in range(qb + 1)]
            prev = None
            pend_final = []
            pend_f2 = []
            for (qb, j) in steps:
                sbs = emit_scores(qb, j)
                if prev is not None:
                    emit_pv(*prev)
                    if prev[1] == prev[0]:
                        pend_final.append(prev[0])
                pi, active = emit_exp(qb, j, sbs)
                while pend_f2:
                    emit_finalize2(pend_f2.pop(0))
                while pend_final:
                    pend_f2.append(emit_finalize1(pend_final.pop(0)))
                prev = (qb, j, pi, active)
            emit_pv(*prev)
            while pend_f2:
                emit_finalize2(pend_f2.pop(0))
            emit_finalize2(emit_finalize1(prev[0]))
            if dbg == 'm4':
                return 'stop'
        if dbg == 'mixA':
            return mix3
        wout_partial(l, 512, mix3)

        sc.barrier()
        AR.off = stage_off
        QT = AR.bf16(S)
        KT = AR.bf16(S)
        JK = AR.f32(128)
        RAW = AR.f32(S + 32)
        ACC = sq[:, :].bitcast(F32)
        KTOK = AR.bf16(16 * 128)
        KTOK3 = KTOK.rearrange("p (t c) -> p t c", c=128)
        GI = AR.f32(64)
        SP_ = AR.f32(64)
        OM = AR.f32(64)
        PSI = AR.f32(64)
        DEC = AR.f32(64)
        NPSI = AR.f32(64)
        NPSI3 = NPSI.rearrange("p (t h) -> p t h", h=4)
        DSEL = AR.f32(16)
        STM = [AR.bf16(128) for _ in range(4)]
        RF = AR.f32(132)
        RBA = AR.bf16(16 * 132)
        RBA3 = RBA.rearrange("p (c k) -> p c k", k=132)
        HRs = [AR.f32(128) for _ in range(4)]
        YTm = [AR.bf16(128) for _ in range(2)]
        GI3 = GI.rearrange("p (t h) -> p t h", h=4)
        SP3 = SP_.rearrange("p (t h) -> p t h", h=4)
        OM3 = OM.rearrange("p (t h) -> p t h", h=4)
        PSI3 = PSI.rearrange("p (t h) -> p t h", h=4)
        DEC3 = DEC.rearrange("p (t h) -> p t h", h=4)
        bg = loadw([(0, 8, 8, w_in_d[l, :, C_IM:C_IM + 8])], 64)
        wg3 = wv(bg, 0, 8, 8)
        gb = 4
        for t in range(16):
            for kc in range(8):
                op('pe', lambda e: e.matmul(psb(gb, t * 8, t * 8 + 8), lhsT=u3[:, kc, t * 128:(t + 1) * 128], rhs=wg3[:, kc, :],
                                            start=(kc == 0), stop=(kc == 7)),
                   R=[('wbf', bg), ures[t // 4]], W=[('ps', gb)], sig=(kc == 7 and t == 15))
        pg3 = psb(gb, 0, 128).rearrange("p (t j) -> p t j", j=8)
        op('dve', lambda e: e.tensor_tensor(out=GI3, in0=pg3[:, :, 0:4], in1=bass.AP(vecs, V_BI + l * 4, [[NV, 128], [0, 16], [1, 4]]), op=ALU.add),
           R=[('ps', gb), 'vecs'], W=['gi'])
        op('dve', lambda e: e.tensor_tensor(out=SP3, in0=pg3[:, :, 4:8], in1=bass.AP(vecs, V_BF + l * 4, [[NV, 128], [0, 16], [1, 4]]), op=ALU.add),
           R=[('ps', gb), 'vecs'], W=['sp'])
        op('act', lambda e: e.activation(out=SP_, in_=SP_, func=AF.Exp, scale=-1.0), R=['sp'], W=['sp'])
        op('act', lambda e: e.activation(out=SP_, in_=SP_, func=AF.Ln, bias=one_c), R=['sp', 'ct'], W=['sp'])
        op('pool', lambda e: e.memset(sd[:, 0:128], 1.0), W=['sd'])
        op('pe', lambda e: e.matmul(psb(5, 0, 64), lhsT=tri, rhs=SP_, start=True, stop=True), R=['sp', 'consts'], W=[('ps', 5)])
        op('pe', lambda e: e.matmul(psb(6, 0, 64), lhsT=sd[:, 0:128], rhs=SP_, start=True, stop=True), R=['sp', 'sd'], W=[('ps', 6)])
        op('dve', lambda e: e.tensor_tensor(out=OM, in0=psb(5, 0, 64), in1=GI, op=ALU.add), R=[('ps', 5), 'gi'], W=['om'])
        op('act', lambda e: e.activation(out=OM, in_=OM, func=AF.Exp), R=['om'], W=['om'])
        op('act', lambda e: e.activation(out=PSI, in_=psb(5, 0, 64), func=AF.Exp, scale=-1.0), R=[('ps', 5)], W=['psi'])
        op('act', lambda e: e.activation(out=DEC, in_=psb(6, 0, 64), func=AF.Exp, scale=-1.0), R=[('ps', 6)], W=['dec'])
        op('dve', lambda e: e.tensor_scalar(out=NPSI, in0=PSI, scalar1=-1.0, scalar2=None, op0=ALU.mult), R=['psi'], W=['psi'])

        for hp in range(2):
            op('pool', lambda e: e.memset(RAW[:, 0:3], 0.0), W=['raw', 'sq'] + [('vpa', c_) for c_ in range(16)] + [('oga', c_) for c_ in range(16)])
            bqk = loadw([(0, 8, 128, w_in_d[l, :, C_QM + hp * 128:C_QM + (hp + 1) * 128]),
                         (1024, 8, 128, w_in_d[l, :, C_KM + hp * 128:C_KM + (hp + 1) * 128])], 2048)
            bvv = loadw([(0, 8, 256, w_in_d[l, :, C_VM + hp * 256:C_VM + (hp + 1) * 256])], 2048)
            boo = loadw([(0, 8, 256, w_in_d[l, :, C_OM + hp * 256:C_OM + (hp + 1) * 256])], 2048)
            wq3 = wv(bqk, 0, 8, 128)
            wk3 = wv(bqk, 1024, 8, 128)
            wv3 = wv(bvv, 0, 8, 256)
            wo3 = wv(boo, 0, 8, 256)
            pr = Ring([4, 5])
            for (w3, dstT, chunk, dres) in ((wq3, QT, hp, 'qt'), (wk3, KT, 2 + hp, 'kt')):
                for blk in range(4):
                    bank = pr.next()
                    for kc in range(8):
                        op('pe', lambda e: e.matmul(psb(bank), lhsT=w3[:, kc, :], rhs=u3[:, kc, blk * 512:(blk + 1) * 512],
                                                    start=(kc == 0), stop=(kc == 7)),
                           R=[('wbf', bqk), ures[blk]], W=[('ps', bank)], sig=(kc == 7))
                    op('act', lambda e: e.copy(out=RAW[:, 3 + blk * 512:3 + (blk + 1) * 512], in_=psb(bank)), R=[('ps', bank)], W=['raw'])
                cw = V_MCW + (l * 4 + chunk) * 4
                op('act', lambda e: e.activation(out=ACC, in_=RAW[:, 3:3 + S], func=AF.Identity, scale=vcol(cw + 3)), R=['raw', 'vecs'], W=['sq'])
                for tap in range(3):
                    op('dve', lambda e: e.scalar_tensor_tensor(out=ACC, in0=RAW[:, tap:tap + S], scalar=vcol(cw + tap), in1=ACC,
                                                               op0=ALU.mult, op1=ALU.add), R=['raw', 'vecs'], W=['sq'])
                op('act', lambda e: e.activation(out=dstT, in_=ACC, func=AF.Silu), R=['sq'], W=[dres])
            for t in range(16):
                if KTOK_DMA:
                    dma(KTOK3[:, t, :], KT[:, t * 128:(t + 1) * 128], R=['kt'], W=['ktok'], key='ktok', transpose=True)
                else:
                    tb = 7
                    pbv = PS[:, tb * 512:tb * 512 + 64].bitcast(BF16)
                    op('pe', lambda e: e.transpose(out=pbv, in_=KT[:, t * 128:(t + 1) * 128], identity=identb), R=['kt', 'cb'], W=[('ps', tb)])
                    op('dve', lambda e: e.tensor_copy(out=KTOK3[:, t, :], in_=pbv), R=[('ps', tb)], W=['ktok'])
            op('pool', lambda e: e.tensor_copy(out=DSEL[0:64, :], in_=DEC3[0:64, :, 2 * hp]), R=['dec'], W=['dsel'])
            op('pool', lambda e: e.tensor_copy(out=DSEL[64:128, :], in_=DEC3[64:128, :, 2 * hp + 1]), R=['dec'], W=['dsel'])
            op('pool', lambda e: e.memset(RF, 0.0), W=['rf'])
            gmb = bass.AP(vecs, V_MNG + l * 128, [[NV, 128], [0, 2], [1, 128]])

            VPA4 = RAW[:, 0:2080].bitcast(BF16).rearrange("p (t j c) -> p t j c", j=2, c=130)
            OGA3 = sq[:, :].rearrange("p (t c) -> p t c", c=256)
            gmb = bass.AP(vecs, V_MNG + l * 128, [[NV, 128], [0, 2], [1, 128]])
            for c in range(16):
                tk = slice(c * 128, (c + 1) * 128)
                bv_, bo_ = (0, 1) if c % 2 == 0 else (2, 3)
                for kc in range(8):
                    op('pe', lambda e: e.matmul(psb(bv_, 0, 256), lhsT=u3[:, kc, tk], rhs=wv3[:, kc, :], start=(kc == 0), stop=(kc == 7)),
                       R=[('wbf', bvv), ures[c // 4]], W=[('ps', bv_)], sig=(kc == 7))
                omb = bass.AP(OM.tensor, OM.offset + c * 4 + 2 * hp, [[ARENA, 128], [1, 2], [0, 128]])
                op('dve', lambda e: e.tensor_tensor(out=VPA4[:, c, :, 0:128], in0=psb(bv_, 0, 256).rearrange("p (j c) -> p j c", c=128), in1=omb, op=ALU.mult),
                   R=[('ps', bv_), 'om', 'raw', 'kt', 'qt'], W=[('vpa', c)])
                op('dve', lambda e: e.tensor_copy(out=VPA4[:, c, :, 128:129], in_=OM3[:, c, 2 * hp:2 * hp + 2].unsqueeze(2)), R=['om', 'raw'], W=[('vpa', c)])
                for kc in range(8):
                    op('pe', lambda e: e.matmul(psb(bo_, 0, 256), lhsT=u3[:, kc, tk], rhs=wo3[:, kc, :], start=(kc == 0), stop=(kc == 7)),
                       R=[('wbf', boo), ures[c // 4]], W=[('ps', bo_)], sig=(kc == 7))
                og_ = OGA3[:, c, :]
                op('act', lambda e: e.activation(out=og_, in_=psb(bo_, 0, 256), func=AF.Sigmoid), R=[('ps', bo_), 'sq', 'kt', 'qt'], W=[('oga', c)])
                op('pool', lambda e: e.tensor_tensor(out=og_.rearrange("p (j c) -> p j c", c=128), in0=og_.rearrange("p (j c) -> p j c", c=128), in1=gmb, op=ALU.mult),
                   R=['vecs'], W=[('oga', c)])

            def stage_a(c):
                tk = slice(c * 128, (c + 1) * 128)
                vi = c % 2
                for j in range(2):
                    pj = slice(j * 64, (j + 1) * 64)
                    sbk = 2 + j
                    op('pe', lambda e: e.matmul(psb(sbk, 0, 128), lhsT=KT[pj, tk], rhs=QT[pj, tk], start=True, stop=True),
                       R=['kt', 'qt'], W=[('ps', sbk)])
                    si_ = vi * 2 + j
                    op('dve', lambda e: e.tensor_tensor(out=STM[si_], in0=psb(sbk, 0, 128), in1=tri, op=ALU.mult),
                       R=[('ps', sbk), 'consts'], W=[('stm', si_)])

            def stage_s(c):
                vi = c % 2
                vp3 = VPA4[:, c]
                for j in range(2):
                    pj = slice(j * 64, (j + 1) * 64)
                    op('pe', lambda e: e.matmul(PS[pj, 512 + 256:512 + 385], lhsT=KTOK3[:, c, j * 64:(j + 1) * 64], rhs=vp3[:, j, 0:129], start=True, stop=True),
                       R=['ktok', ('vpa', c)], W=[('ps', 1)], sig=(j == 1))
                op('dve', lambda e: e.tensor_tensor(out=RF[:, 0:129], in0=psb(1, 256, 385), in1=RF[:, 0:129], op=ALU.add), R=[('ps', 1)], W=['rf'])
                op('dve', lambda e: e.tensor_scalar(out=RF[:, 0:129], in0=RF[:, 0:129], scalar1=DSEL[:, c:c + 1], scalar2=None, op0=ALU.mult),
                   R=['dsel'], W=['rf'])
                op('act', lambda e: e.copy(out=RBA3[:, c + 1, 0:129], in_=RF[:, 0:129]), R=['rf'], W=[('rba', c + 1)])

            def stage_b(c):
                tk = slice(c * 128, (c + 1) * 128)
                vi = c % 2
                vp3 = VPA4[:, c]
                og3 = OGA3[:, c, :].rearrange("p (j c) -> p j c", c=128)
                nbase = 4 + 2 * (c % 2)
                for j in range(2):
                    pj = slice(j * 64, (j + 1) * 64)
                    si_ = vi * 2 + j
                    nb_ = nbase + j
                    op('pe', lambda e: e.matmul(psb(nb_, 0, 129), lhsT=STM[si_], rhs=vp3[:, j, 0:129], start=True, stop=(c == 0)),
                       R=[('stm', si_), ('vpa', c)], W=[('ps', nb_)], sig=(c == 0))
                    if c > 0:
                        op('pe', lambda e: e.matmul(psb(nb_, 0, 129), lhsT=QT[pj, tk], rhs=RBA3[pj, c, 0:129], start=False, stop=True),
                           R=['qt', ('rba', c)], W=[('ps', nb_)])
                k0 = sm_ring.next() * 8
                r = sm[:, k0:k0 + 8]
                rres = ('smr', k0)
                den2 = bass.AP(PS, nbase * 512 + 128, [[4096, 128], [512, 2]])
                psi2 = PSI3[:, c, 2 * hp:2 * hp + 2]
                nres = [('ps', nbase), ('ps', nbase + 1)]
                op('dve', lambda e: e.tensor_tensor(out=r[:, 0:2], in0=den2, in1=psi2, op=ALU.mult), R=nres + ['psi'], W=[rres])
                op('dve', lambda e: e.scalar_tensor_tensor(out=r[:, 2:4], in0=r[:, 0:2], scalar=-1.0, in1=r[:, 0:2], op0=ALU.mult, op1=ALU.max), R=[rres], W=[rres])
                op('dve', lambda e: e.tensor_scalar(out=r[:, 2:4], in0=r[:, 2:4], scalar1=8.0, scalar2=None, op0=ALU.max), R=[rres], W=[rres])
                op('dve', lambda e: e.reciprocal(out=r[:, 4:6], in_=r[:, 2:4]), R=[rres], W=[rres])
                op('dve', lambda e: e.tensor_tensor(out=r[:, 4:6], in0=r[:, 4:6], in1=psi2, op=ALU.mult), R=['psi'], W=[rres])
                for j in range(2):
                    nb_ = nbase + j
                    hj = vi * 2 + j
                    op('act', lambda e: e.activation(out=HRs[hj], in_=psb(nb_, 0, 128), func=AF.Identity, scale=r[:, 4 + j:5 + j]), R=[('ps', nb_), rres], W=[('hr', hj)])
                    op('act', lambda e: e.activation(out=JK, in_=HRs[hj], func=AF.Square, accum_out=r[:, 6 + j:7 + j]), R=[('hr', hj)], W=['jk', (rres, 6 + j)])
                tiny_rstd(r[:, 6:8], r[:, 6:8], 1.0 / 128.0, EPS, [(rres, 6), (rres, 7)], [(rres, 'rs')], nh=nhalf2)

                def part2():
                    for j in range(2):
                        hd = 2 * hp + j
                        hj = vi * 2 + j
                        yi = yt_ring.next()
                        op('dve', lambda e: e.scalar_tensor_tensor(out=YTm[yi], in0=HRs[hj], scalar=r[:, 6 + j:7 + j], in1=og3[:, j, :], op0=ALU.mult, op1=ALU.mult),
                           R=[('hr', hj), (rres, 'rs'), ('oga', c)], W=[('ytm', yi)])
                        dma(mix3[:, hd, tk], YTm[yi], R=[('ytm', yi)], W=[('mix', hd, c)], key=('ytmd', yi), transpose=True)
                return part2

            stage_a(0)
            stage_s(0)
            pend2 = None
            for c in range(16):
                if c + 1 < 16:
                    stage_a(c + 1)
                    if c + 1 < 15:
                        stage_s(c + 1)
                p2 = stage_b(c)
                if pend2 is not None:
                    pend2()
                pend2 = p2
            pend2()
        if dbg == 'mixB':
            return mix3
        wout_partial(l, 0, mix3)
        return None

    def ffn(l, s):
        AR.reset()
        U = AR.bf16(8 * 1024)
        U3 = U.rearrange("p (c t) -> p c t", t=1024)
        ACTT = AR.bf16(22 * 1024)
        A3 = ACTT.rearrange("p (c t) -> p c t", t=1024)
        G = [AR.f32(1024) for _ in range(2)]
        GG = [AR.bf16(1024) for _ in range(2)]
        GV = [AR.f32(1024) for _ in range(2)]
        HB = AR.f32(96)
        HB3 = HB.rearrange("p (c t) -> p c t", t=2)
        op('pool', lambda e: e.memset(HB, 0.0), W=['hb'])
        for half in range(2):
            norm(V_GFFN + l * 8, half * 1024, 1024, U3, 'uf')
            specs = []
            for cp in range(22):
                specs.append(([(0, 8, 128, w_up_d[l, :, cp * 128:(cp + 1) * 128]),
                               (1024, 8, 128, w_up_d[l, :, DFF + cp * 128:DFF + (cp + 1) * 128])], 2048))
            for oc in range(8):
                specs.append(([(0, 11, 128, w_dn_d[l, 0:1408, oc * 128:(oc + 1) * 128])], 1408))
                specs.append(([(0, 11, 128, w_dn_d[l, 1408:2816, oc * 128:(oc + 1) * 128])], 1408))
            ws = WStream(specs, look=2, eng='act')
            for cp in range(22):
                bi = ws.get(cp)
                gi_ = cp % 2
                for gv in range(2):
                    ch = cp + 22 * gv
                    b0 = 3 * gv
                    w3 = wv(bi, 1024 * gv, 8, 128)
                    op('act', lambda e: e.copy(out=psb(b0, 510, 512), in_=HB3[:, ch, :]), R=['hb'], W=[('ps', b0)])
                    for tb in range(2):
                        for kc in range(8):
                            op('pe', lambda e: e.matmul(psb(b0 + 1 + tb), lhsT=w3[:, kc, :], rhs=U3[:, kc, tb * 512:(tb + 1) * 512],
                                                        start=(kc == 0), stop=(kc == 7)),
                               R=[('wbf', bi), ('uf', tb)], W=[('ps', b0 + 1 + tb)], sig=(kc == 7))
                    if half == 0:
                        op('act', lambda e: e.copy(out=HB3[:, ch, :], in_=psb(b0 + 2, 510, 512)), R=[('ps', b0 + 2)], W=['hb'])
                    X0 = b0 * 512 + 510
                    pres = [('ps', b0), ('ps', b0 + 1), ('ps', b0 + 2)]
                    cw = V_FCW + (l * 44 + ch) * 3
                    cbias = V_FCB + l * 44 + ch
                    dst = G[gi_] if gv == 0 else GV[gi_]
                    dres = ('g', gi_) if gv == 0 else ('gv', gi_)
                    op('act', lambda e: e.activation(out=dst, in_=PS[:, X0 + 2:X0 + 1026], func=AF.Identity, scale=vcol(cw + 2), bias=vcol(cbias)),
                       R=pres + ['vecs'], W=[dres])
                    op('dve', lambda e: e.scalar_tensor_tensor(out=dst, in0=PS[:, X0 + 1:X0 + 1025], scalar=vcol(cw + 1), in1=dst, op0=ALU.mult, op1=ALU.add),
                       R=pres + ['vecs'], W=[dres])
                    op('dve', lambda e: e.scalar_tensor_tensor(out=dst, in0=PS[:, X0:X0 + 1024], scalar=vcol(cw), in1=dst, op0=ALU.mult, op1=ALU.add),
                       R=pres + ['vecs'], W=[dres])
                    if gv == 0:
                        op('act', lambda e: e.activation(out=GG[gi_], in_=dst, func=AF.Gelu), R=[dres], W=[('gg', gi_)])
                    else:
                        op('pool', lambda e: e.tensor_tensor(out=A3[:, cp, :], in0=GG[gi_], in1=dst, op=ALU.mult),
                           R=[('gg', gi_), dres], W=[('a', cp)])
            ring = Ring([6, 7])
            ares = [('a', cp) for cp in range(22)]
            for oc in range(8):
                b1 = ws.get(22 + 2 * oc)
                b2 = ws.get(22 + 2 * oc + 1)
                wd = [wv(b1, 0, 11, 128), wv(b2, 0, 11, 128)]
                for tb in range(2):
                    bank = ring.next()
                    for kc in range(22):
                        op('pe', lambda e: e.matmul(psb(bank), lhsT=wd[kc // 11][:, kc % 11, :], rhs=A3[:, kc, tb * 512:(tb + 1) * 512],
                                                    start=(kc == 0), stop=(kc == 21)),
                           R=[('wbf', b1), ('wbf', b2), ares[kc]], W=[('ps', bank)], sig=(kc == 21))
                    blk = half * 2 + tb
                    dst = hT3[:, oc, blk * 512:(blk + 1) * 512]
                    op('dve', lambda e: e.tensor_tensor(out=dst, in0=psb(bank), in1=dst, op=ALU.add), R=[('ps', bank)], W=[('h', blk)])

    def ple(l, s):
        AR.reset()
        uT = AR.bf16(8 * S)
        u3 = uT.rearrange("p (c t) -> p c t", t=S)
        PTt = AR.bf16(2 * S)
        PT3 = PTt.rearrange("p (c t) -> p c t", t=S)
        pst = [AR.f32(256), AR.f32(256)]
        SG = [AR.f32(512), AR.f32(512)]
        TT = [AR.f32(512), AR.f32(512)]
        norm(V_GPLE + l * 8, 0, S, u3, 'u')
        for t in range(16):
            b = t % 2
            dma(pst[b], p_d[l, s, t * 128:(t + 1) * 128, :], W=[('pst', b)])
            bank = psX.next()
            for c in range(2):
                op('pe', lambda e: e.transpose(out=psb(bank, c * 128, c * 128 + 128), in_=pst[b][:, c * 128:(c + 1) * 128], identity=identf),
                   R=[('pst', b), 'consts'], W=[('ps', bank)], sig=(c == 1))
            op('act', lambda e: e.copy(out=PT3[:, :, t * 128:(t + 1) * 128], in_=psb(bank, 0, 256).rearrange("p (c t) -> p c t", t=128)),
               R=[('ps', bank)], W=['ptt'])
        WP = AR.bf16(2048)
        loadw([(0, 2, 1024, w_pp_d[l, :, :])], 2048, dst_ap=WP, dst_res='wp')
        wp3 = WP.rearrange("p (k c) -> p k c", c=1024)
        specs = []
        for o2 in range(4):
            specs.append(([(0, 8, 256, w_pg_d[l, :, o2 * 256:(o2 + 1) * 256])], 2048))
        ws = WStream(specs, look=2)
        ring = Ring([4, 5, 6, 7])
        k = 0
        for o2 in range(4):
            bg_ = ws.get(o2)
            wg3 = wv(bg_, 0, 8, 256)
            for o1 in range(2):
                oc = o2 * 2 + o1
                for blk in range(4):
                    ba = ring.next()
                    bb = ring.next()
                    for kc in range(8):
                        op('pe', lambda e: e.matmul(psb(ba), lhsT=wg3[:, kc, o1 * 128:(o1 + 1) * 128], rhs=u3[:, kc, blk * 512:(blk + 1) * 512],
                                                    start=(kc == 0), stop=(kc == 7)),
                           R=[('wbf', bg_), ('u', blk)], W=[('ps', ba)], sig=(kc == 7))
                    for kc in range(2):
                        op('pe', lambda e: e.matmul(psb(bb), lhsT=wp3[:, kc, oc * 128:(oc + 1) * 128], rhs=PT3[:, kc, blk * 512:(blk + 1) * 512],
                                                    start=(kc == 0), stop=(kc == 1)),
                           R=['wp', 'ptt'], W=[('ps', bb)], sig=(kc == 1))
                    i = k % 2
                    k += 1
                    op('act', lambda e: e.activation(out=SG[i], in_=psb(ba), func=AF.Sigmoid), R=[('ps', ba)], W=[('sg', i)])
                    op('dve', lambda e: e.tensor_tensor(out=TT[i], in0=psb(bb), in1=SG[i], op=ALU.mult), R=[('ps', bb), ('sg', i)], W=[('tt', i)])
                    dst = hT3[:, oc, blk * 512:(blk + 1) * 512]
                    op('pool', lambda e: e.tensor_tensor(out=dst, in0=dst, in1=TT[i], op=ALU.add), R=[('tt', i)], W=[('h', blk)])

    def dump(ap3_or_none):
        sc.barrier()
        if ap3_or_none is None:
            dma(dbg_d[:, :], hT[:, :], R=[('h', b) for b in range(4)], key='dbg')
        else:
            n = ap3_or_none.shape[1] * ap3_or_none.shape[2]
            tmpf = arena[:, ARENA - 4 * S:ARENA]
            op('dve', lambda e: e.tensor_copy(out=tmpf[:, 0:n], in_=ap3_or_none.rearrange("p c t -> p (c t)")), W=['dbgt'])
            dma(dbg_d[:, 0:n], tmpf[:, 0:n], R=['dbgt'], key='dbg')
        sc.finish()

    for s in range(nseq):
        sc.barrier()
        phase_load(s)
        if dbg == 'x':
            dump(None)
            return nc
        for l in range(nlayer):
            sc.barrier()
            r = mixer(l, s)
            if r == 'stop':
                dump(None)
                return nc
            if dbg in ('mixA', 'mixB'):
                dump(r)
                return nc
            if dbg == 'mix':
                dump(None)
                return nc
            sc.barrier()
            ffn(l, s)
            if dbg == 'ffn':
                dump(None)
                return nc
            sc.barrier()
            ple(l, s)
            if dbg == 'ple':
                dump(None)
                return nc
        sc.barrier()
        phase_store(s)
    sc.finish()
    return nc


def _bucket_table():
    n = np.arange(4096)
    max_exact = 16
    nf = np.maximum(n, 1).astype(np.float32)
    large = max_exact + (np.log(nf / np.float32(max_exact)) / np.float32(math.log(128 / max_exact)) * np.float32(16)).astype(np.int32)
    large = np.minimum(large, 31)
    return np.where(n < max_exact, n, large)


def make_consts():
    c = np.zeros((128, NCONST), np.float32)
    c[:, K_ID:K_ID + 128] = np.eye(128, dtype=np.float32)
    i = np.arange(128)
    c[:, K_TRI:K_TRI + 128] = (i[:, None] <= i[None, :]).astype(np.float32)
    c[:, K_MNEG:K_MNEG + 128] = np.where(i[:, None] <= i[None, :], 0.0, NEG).astype(np.float32)
    bk = _bucket_table()
    lo = np.full(32, 1e9, np.float32)
    for b in range(31, -1, -1):
        idx = np.nonzero(bk == b)[0]
        lo[b] = idx[0] if len(idx) else (lo[b + 1] if b < 31 else 1e9)
    hi = np.concatenate([lo[1:], np.array([1e9], np.float32)])
    lo[0] = -1e9
    c[0:32, K_THR] = lo
    c[0:32, K_THR + 1] = hi
    return c


def make_vecs(inp):
    v = np.zeros((128, NV), np.float32)

    def fm(vec):
        return np.ascontiguousarray(vec.reshape(-1, 128).T)

    for l in range(DEPTH):
        v[:, V_GMIX + l * 8:V_GMIX + l * 8 + 8] = fm(inp['ln_mix_g'][l])
        v[:, V_GFFN + l * 8:V_GFFN + l * 8 + 8] = fm(inp['ln_ffn_g'][l])
        v[:, V_GPLE + l * 8:V_GPLE + l * 8 + 8] = fm(inp['ln_ple_g'][l])
        cw = inp['ffn_conv_w'][l]
        for tap in range(3):
            t_ = fm(cw[tap])
            v[:, V_FCW + l * 132 + tap:V_FCW + (l + 1) * 132:3] = t_
        v[:, V_FCB + l * 44:V_FCB + (l + 1) * 44] = fm(inp['ffn_conv_b'][l])
        mw = inp['mlstm_conv_w'][l]
        for tap in range(4):
            t_ = fm(mw[tap])
            v[:, V_MCW + l * 16 + tap:V_MCW + (l + 1) * 16:4] = t_
        v[:, V_GQ + l] = np.tile(inp['q_norm_g'][l], 2)
        v[:, V_GK + l] = np.tile(inp['k_norm_g'][l], 2)
        v[:, V_MNG + l * 128:V_MNG + (l + 1) * 128] = np.broadcast_to(inp['mlstm_norm_g'][l][None, :], (128, 128))
        v[:, V_SLG + l * 128:V_SLG + (l + 1) * 128] = np.broadcast_to(inp['diff_subln_g'][l][None, :], (128, 128))
        v[:, V_BI + l * 4:V_BI + (l + 1) * 4] = np.broadcast_to(inp['b_igate'][l][None, :], (128, 4))
        v[:, V_BF + l * 4:V_BF + (l + 1) * 4] = np.broadcast_to(inp['b_fgate'][l][None, :], (128, 4))
        for k, name in enumerate(('lam_q1', 'lam_k1', 'lam_q2', 'lam_k2')):
            v[:, V_LAM + l * 256 + k * 64:V_LAM + l * 256 + (k + 1) * 64] = np.broadcast_to(inp[name][l][None, :], (128, 64))
    v[0:32, V_RB:V_RB + 8] = inp['rel_bias'].reshape(32, 8)
    return v


def make_in_maps(inp, ncore=NCORE):
    f = lambda a: np.ascontiguousarray(np.asarray(a, dtype=np.float32))
    consts = make_consts()
    vecs = make_vecs({k: np.asarray(v) for k, v in inp.items()})
    shared = {
        'w_in': f(inp['w_in']), 'w_out': f(inp['w_out']), 'w_up': f(inp['w_up']), 'w_down': f(inp['w_down']),
        'w_ple_gate': f(inp['w_ple_gate']), 'w_ple_proj': f(inp['w_ple_proj']), 'consts': consts, 'vecs': vecs,
        'positions': np.ascontiguousarray(np.asarray(inp['positions'], dtype=np.int32)),
    }
    x = f(inp['x'])
    p = f(inp['p'])
    maps = []
    for c in range(ncore):
        m = dict(shared)
        m['x'] = np.ascontiguousarray(x[2 * c:2 * c + 2])
        m['p'] = np.ascontiguousarray(p[:, 2 * c:2 * c + 2])
        maps.append(m)
    return maps


def kernel(**inputs):
    nc = build()
    maps = make_in_maps(inputs)
    res = run_bass_kernel_spmd(nc, maps, core_ids=list(range(NCORE)))
    out = np.concatenate([np.asarray(r['out']) for r in res.results], axis=0)
    return out.astype(np.float32)
```
